# Optimizing a Trainium2 kernel written in Bass

```python
import math
import jax, jax.numpy as jnp
from jax import lax
import numpy as np


D_MODEL = 2048
BATCH = 2
SEQ = 8192
DEPTH = 2

N_HEADS = 8
HEAD_DIM = 64
ATTN_WIDTH = N_HEADS * 2 * HEAD_DIM
POOL_WINDOWS = (2, 4, 8, 16)
POOL_GROUP = 256
POOL_WIDTH = POOL_GROUP * len(POOL_WINDOWS)
MAX_WINDOW = max(POOL_WINDOWS)
N_BUCKETS = 32
MAX_DISTANCE = 128
BLOCK_Q = 128
D_FF_DENSE = 5632
N_EXPERTS = 8
TOP_K = 2
D_FF_EXPERT = 7168
ALPHA = (2.0 * DEPTH) ** 0.25
BETA = (8.0 * DEPTH) ** -0.25
LN_EPS = 1e-5
N_DENSE = (DEPTH + 1) // 2
N_MOE = DEPTH // 2
IN_COLS = (ATTN_WIDTH, ATTN_WIDTH, ATTN_WIDTH, POOL_WIDTH, D_MODEL, D_MODEL)
IN_WIDTH = sum(IN_COLS)
SPLIT_POINTS = tuple(int(s) for s in np.cumsum(IN_COLS)[:-1])

kernel_name = 'hybrid_diffattn_pool_moe_block'


def layer_norm(x, g, b):
    xf = x.astype(jnp.float32)
    mu = jnp.mean(xf, axis=-1, keepdims=True)
    var = jnp.mean(jnp.square(xf - mu), axis=-1, keepdims=True)
    return ((xf - mu) * lax.rsqrt(var + LN_EPS) * g.astype(jnp.float32) + b.astype(jnp.float32)).astype(x.dtype)


def t5_causal_bucket(q_pos, k_pos):
    n = jnp.maximum(q_pos[:, None] - k_pos[None, :], 0)
    max_exact = N_BUCKETS // 2
    large = max_exact + (jnp.log(jnp.maximum(n, 1).astype(jnp.float32) / max_exact)
                         / math.log(MAX_DISTANCE / max_exact) * (N_BUCKETS - max_exact)).astype(jnp.int32)
    large = jnp.minimum(large, N_BUCKETS - 1)
    return jnp.where(n < max_exact, n, large)


def diff_attention(q, k, v, rel_bias, lam):
    B, S = q.shape[0], q.shape[1]
    k_pos = jnp.arange(S)
    scale = HEAD_DIM ** -0.5

    def block(i):
        start = i * BLOCK_Q
        qb = lax.dynamic_slice_in_dim(q, start, BLOCK_Q, axis=1)
        q_pos = start + jnp.arange(BLOCK_Q)
        s = jnp.einsum('bqhcd,bkhcd->bchqk', qb, k).astype(jnp.float32) * scale
        bias = jnp.transpose(rel_bias[t5_causal_bucket(q_pos, k_pos)].astype(jnp.float32), (2, 0, 1))
        causal = k_pos[None, :] <= q_pos[:, None]
        s = jnp.where(causal, s + bias, -jnp.inf)
        p = jax.nn.softmax(s, axis=-1)
        a = p[:, 0] - lam * p[:, 1]
        return jnp.einsum('bhqk,bkhe->bqhe', a.astype(v.dtype), v)

    o = lax.map(block, jnp.arange(S // BLOCK_Q))
    return jnp.moveaxis(o, 0, 1).reshape(B, S, N_HEADS, 2 * HEAD_DIM)


def multiscale_pool(u, pool_w, pool_scale):
    B, S, _ = u.shape
    ug = u.astype(jnp.float32).reshape(B, S, len(POOL_WINDOWS), POOL_GROUP)
    c = jnp.cumsum(ug, axis=1)
    c = jnp.concatenate([jnp.zeros((B, MAX_WINDOW) + c.shape[2:], jnp.float32), c], axis=1)
    t = jnp.arange(S)
    means = []
    for g, w in enumerate(POOL_WINDOWS):
        win_sum = c[:, MAX_WINDOW:, g] - c[:, MAX_WINDOW - w:MAX_WINDOW - w + S, g]
        cnt = jnp.minimum(t + 1, w).astype(jnp.float32)
        means.append(win_sum / cnt[None, :, None])
    z = (jnp.stack(means, axis=2) - ug).astype(u.dtype)
    y = jnp.einsum('bsgc,gce->bsge', z, pool_w).reshape(B, S, POOL_WIDTH)
    return y * pool_scale


def hybrid_mixer(h, w_in, lam_vec, subln_w, pool_w, pool_scale, w_ba, w_bp, w_out, rel_bias, lambda_init):
    B, S, _ = h.shape
    proj = h @ w_in
    q, k, v, u, ga, gp = jnp.split(proj, SPLIT_POINTS, axis=-1)
    q = q.reshape(B, S, N_HEADS, 2, HEAD_DIM)
    k = k.reshape(B, S, N_HEADS, 2, HEAD_DIM)
    v = v.reshape(B, S, N_HEADS, 2 * HEAD_DIM)
    lv = lam_vec.astype(jnp.float32)
    lam = jnp.exp(jnp.sum(lv[0] * lv[1])) - jnp.exp(jnp.sum(lv[2] * lv[3])) + lambda_init
    o = diff_attention(q, k, v, rel_bias, lam).astype(jnp.float32)
    o = o * lax.rsqrt(jnp.mean(jnp.square(o), axis=-1, keepdims=True) + LN_EPS) * subln_w.astype(jnp.float32) * (1.0 - lambda_init)
    attn_out = o.reshape(B, S, ATTN_WIDTH).astype(h.dtype)
    pool_out = multiscale_pool(u, pool_w, pool_scale)
    merged = jax.nn.sigmoid(ga) * (attn_out @ w_ba) + jax.nn.sigmoid(gp) * (pool_out @ w_bp)
    return merged @ w_out


def dense_swiglu(h, wg, wu, wd):
    return (jax.nn.silu(h @ wg) * (h @ wu)) @ wd


def moe_swiglu(h, router_w, wg, wu, wd):
    B, S, D = h.shape
    t = h.reshape(B * S, D)
    logits = (t @ router_w).astype(jnp.float32)
    top_v, top_i = lax.top_k(logits, TOP_K)
    top_w = jax.nn.softmax(top_v, axis=-1)
    gates = jnp.sum(jax.nn.one_hot(top_i, N_EXPERTS, dtype=jnp.float32) * top_w[..., None], axis=1)
    y = jnp.zeros((B * S, D), jnp.float32)
    for e in range(N_EXPERTS):
        he = jax.nn.silu(t @ wg[e]) * (t @ wu[e])
        y = y + gates[:, e:e + 1] * (he @ wd[e]).astype(jnp.float32)
    return y.reshape(B, S, D).astype(h.dtype)


def setup_inputs(seed: int = 0) -> dict:
    key = jax.random.key(seed)
    ks = jax.random.split(key, 24)

    def nrm(k, shape, s):
        return jax.random.normal(k, shape, jnp.float32) * s

    return {
        'x': nrm(ks[0], (BATCH, SEQ, D_MODEL), 1.0),
        'w_in': nrm(ks[1], (DEPTH, D_MODEL, IN_WIDTH), D_MODEL ** -0.5),
        'lambdas': nrm(ks[2], (DEPTH, 4, HEAD_DIM), 0.1),
        'subln_w': 1.0 + nrm(ks[3], (DEPTH, 2 * HEAD_DIM), 0.02),
        'pool_w': nrm(ks[4], (DEPTH, len(POOL_WINDOWS), POOL_GROUP, POOL_GROUP), POOL_GROUP ** -0.5),
        'pool_scale': 1.0 + nrm(ks[5], (DEPTH, POOL_WIDTH), 0.02),
        'w_branch_attn': nrm(ks[6], (DEPTH, ATTN_WIDTH, D_MODEL), ATTN_WIDTH ** -0.5),
        'w_branch_pool': nrm(ks[7], (DEPTH, POOL_WIDTH, D_MODEL), POOL_WIDTH ** -0.5),
        'w_out': nrm(ks[8], (DEPTH, D_MODEL, D_MODEL), BETA * D_MODEL ** -0.5),
        'rel_bias': nrm(ks[9], (N_BUCKETS, N_HEADS), 0.5),
        'ln1_g': 1.0 + nrm(ks[10], (DEPTH, D_MODEL), 0.02),
        'ln1_b': nrm(ks[11], (DEPTH, D_MODEL), 0.02),
        'dense_w_gate': nrm(ks[12], (N_DENSE, D_MODEL, D_FF_DENSE), D_MODEL ** -0.5),
        'dense_w_up': nrm(ks[13], (N_DENSE, D_MODEL, D_FF_DENSE), D_MODEL ** -0.5),
        'dense_w_down': nrm(ks[14], (N_DENSE, D_FF_DENSE, D_MODEL), BETA * D_FF_DENSE ** -0.5),
        'router_w': nrm(ks[15], (N_MOE, D_MODEL, N_EXPERTS), D_MODEL ** -0.5),
        'moe_w_gate': nrm(ks[16], (N_MOE, N_EXPERTS, D_MODEL, D_FF_EXPERT), D_MODEL ** -0.5),
        'moe_w_up': nrm(ks[17], (N_MOE, N_EXPERTS, D_MODEL, D_FF_EXPERT), D_MODEL ** -0.5),
        'moe_w_down': nrm(ks[18], (N_MOE, N_EXPERTS, D_FF_EXPERT, D_MODEL), BETA * D_FF_EXPERT ** -0.5),
        'ln2_g': 1.0 + nrm(ks[19], (DEPTH, D_MODEL), 0.02),
        'ln2_b': nrm(ks[20], (DEPTH, D_MODEL), 0.02),
    }


def reference(x, w_in, lambdas, subln_w, pool_w, pool_scale, w_branch_attn, w_branch_pool, w_out, rel_bias,
              ln1_g, ln1_b, dense_w_gate, dense_w_up, dense_w_down, router_w, moe_w_gate, moe_w_up, moe_w_down,
              ln2_g, ln2_b):
    for l in range(DEPTH):
        lambda_init = 0.8 - 0.6 * math.exp(-0.3 * l)
        mix = hybrid_mixer(x, w_in[l], lambdas[l], subln_w[l], pool_w[l], pool_scale[l],
                           w_branch_attn[l], w_branch_pool[l], w_out[l], rel_bias, lambda_init)
        x = layer_norm(ALPHA * x + mix, ln1_g[l], ln1_b[l])
        if l % 2 == 0:
            f = dense_swiglu(x, dense_w_gate[l // 2], dense_w_up[l // 2], dense_w_down[l // 2])
        else:
            f = moe_swiglu(x, router_w[l // 2], moe_w_gate[l // 2], moe_w_up[l // 2], moe_w_down[l // 2])
        x = layer_norm(ALPHA * x + f, ln2_g[l], ln2_b[l])
    return x
```

```python
import math
import numpy as np
import concourse.bass as bass
import concourse.mybir as mybir
from concourse.bass_utils import run_bass_kernel_spmd

F32, BF16 = mybir.dt.float32, mybir.dt.bfloat16
AF = mybir.ActivationFunctionType
ALU = mybir.AluOpType
AX = mybir.AxisListType
NEG = -30000.0


class Cfg:
    def __init__(s, D=2048, P=8192, OWN=2048, H=8, PG=256, DFF=5632, NE=8, DFE=7168, DEPTH=2, BATCH=2):
        s.D, s.P, s.OWN, s.H, s.PG, s.DFF, s.NE, s.DFE, s.DEPTH, s.BATCH = D, P, OWN, H, PG, DFF, NE, DFE, DEPTH, BATCH
        s.AW = H * 128
        s.PW = 4 * PG
        s.INW = 3 * s.AW + s.PW + 2 * D
        s.ALPHA = (2.0 * DEPTH) ** 0.25
        s.GW = 1152


class Sem:
    def __init__(s, nc, name):
        s.h = nc.alloc_semaphore(name=name)
        s.n = 0

    def inc(s, ins, k=1):
        ins.then_inc(s.h, k)
        s.n += k
        return s.n


class Chain:
    def __init__(s, p, name):
        s.s = p.sem(name)

    def go(s, eng, fn, k=1, extra=()):
        if s.s.n:
            eng.wait_ge(s.s.h, s.s.n)
        for (sm, v) in extra:
            if v:
                eng.wait_ge(sm.h, v)
        ins = fn()
        s.s.inc(ins, k)
        return ins

    def dma(s, eng, out, in_, extra=(), **kw):
        return s.go(eng, lambda: eng.dma_start(out=out, in_=in_, **kw), 16, extra)

    def wait(s, eng):
        if s.s.n:
            eng.wait_ge(s.s.h, s.s.n)


class Prog:
    def __init__(p, cfg):
        p.cfg = cfg
        p.nc = bass.Bass("TRN2", target_bir_lowering=False)
        p.uid = 0

    def din(p, name, shape, dt=F32):
        return p.nc.dram_tensor(name, list(shape), dt, kind="ExternalInput").ap()

    def dram(p, name, shape, dt):
        return p.nc.dram_tensor(name, list(shape), dt).ap()

    def sb(p, name, shape, dt):
        p.uid += 1
        return p.nc.alloc_sbuf_tensor(f"{name}_{p.uid}", list(shape), dt)

    def sem(p, name):
        p.uid += 1
        return Sem(p.nc, f"{name}_{p.uid}")


def fm(ap):
    return ap.rearrange("(kc q) n -> q kc n", q=128)


def bcast_row(ap_row, n):
    return bass.AP(ap_row.tensor, ap_row.offset, [[0, 128], [1, n]])


def gemm(p, XT, K, t0, ntok, Ws, M, epi, dst, NTP, out_dt=BF16, tokmajor=False):
    nc = p.nc
    Kc = K // 128
    nW = len(Ws)
    nb = NTP // 512
    assert nW * nb <= 4 and ntok % NTP == 0
    npass = ntok // NTP
    with nc.cleanup_on_exit():
        xs = p.sb("xs", [128, Kc, NTP], BF16)
        MWd = 512 if tokmajor else 128
        ws = [[p.sb("ws", [128, Kc, MWd], BF16) for _ in range(nW)] for _ in range(2)]
        outs = [p.sb("ot", [128, (512 if tokmajor else NTP)], out_dt) for _ in range(2)]
        tmps = [p.sb("tp", [128, 512], F32) for _ in range(2)]
        ch = [Chain(p, "ga"), Chain(p, "gb")]
        cx = p.sem("cx")
        XTv = fm(XT)
        Wv = [fm(w) for w in Ws]
        u = 0
        for pi in range(npass):
            ch[0].wait(nc.sync)
            ch[1].wait(nc.sync)
            cx.inc(nc.sync.dma_start(out=xs[:], in_=XTv[:, :, t0 + pi * NTP: t0 + (pi + 1) * NTP]), 16)
            if tokmajor:
                units = [(si, tb) for si in range(M // 512) for tb in range(NTP // 128)]
            else:
                units = [(mi, 0) for mi in range(M // 128)]
            prev_si = [None, None]
            for (a, b) in units:
                c = u % 2
                C = ch[c]
                b0 = 4 * c
                if tokmajor:
                    for w in range(nW):
                        C.dma(nc.gpsimd, ws[c][w][:], Wv[w][:, :, a * 512:(a + 1) * 512])
                    C.wait(nc.tensor)
                    nc.tensor.wait_ge(cx.h, cx.n)
                    for kc in range(Kc):
                        ins = nc.tensor.matmul(p.ps[:, b0, :], xs[:, kc, b * 128:(b + 1) * 128], ws[c][0][:, kc, :],
                                               start=(kc == 0), stop=(kc == Kc - 1))
                    C.s.inc(ins)
                    epi(C, [p.ps[:, b0, :]], outs[c][:], tmps[c], pi, a, b)
                    d = dst(pi, a, b)
                    C.dma(nc.sync, d, outs[c][:])
                else:
                    for w in range(nW):
                        C.dma(nc.gpsimd, ws[c][w][:], Wv[w][:, :, a * 128:(a + 1) * 128])
                    C.wait(nc.tensor)
                    nc.tensor.wait_ge(cx.h, cx.n)
                    for w in range(nW):
                        for j in range(nb):
                            for kc in range(Kc):
                                ins = nc.tensor.matmul(p.ps[:, b0 + w * nb + j, :], ws[c][w][:, kc, :],
                                                       xs[:, kc, j * 512:(j + 1) * 512],
                                                       start=(kc == 0), stop=(kc == Kc - 1))
                    C.s.inc(ins)
                    for j in range(nb):
                        epi(C, [p.ps[:, b0 + w * nb + j, :] for w in range(nW)], outs[c][:, j * 512:(j + 1) * 512],
                            tmps[c], pi, a, j)
                    if dst is not None:
                        C.dma(nc.sync, dst(a, pi), outs[c][:])
                u += 1
        for e in (nc.sync, nc.gpsimd, nc.tensor, nc.vector, nc.scalar):
            ch[0].wait(e)
            ch[1].wait(e)
        nc.all_engine_barrier()


def gemm2(p, XT, K, t0, ntok, Ws, M, body, dsts, NTP, out_dt=BF16, auxsrcs=None, aux_dts=(), pass_aux=None):
    nc = p.nc
    Kc = K // 128
    nW = len(Ws)
    NTP = min(NTP, ntok)
    nbs = min(2 // nW, NTP // 512)
    assert ntok % NTP == 0 and NTP % (512 * nbs) == 0
    npass = ntok // NTP
    nblk = NTP // 512
    nch = 4 if Kc <= 16 else 2
    nax = len(aux_dts)
    osz = 2 if out_dt == BF16 else 4
    asz = sum(2 if d == BF16 else 4 for d in aux_dts)
    est = lambda wb: Kc * NTP * 2 + nch * wb * nW * Kc * 256 + nch * NTP * (osz + asz) + nch * 2048 + (NTP * 4 if pass_aux else 0)
    wbuf = 2 if est(2) <= 148 * 1024 else 1
    nM = M // 128
    with nc.cleanup_on_exit():
        xs = p.sb("xs", [128, Kc, NTP], BF16)
        ws = [[[p.sb("ws", [128, Kc, 128], BF16) for _ in range(nW)] for _ in range(wbuf)] for _ in range(nch)]
        outs = [p.sb("ot", [128, NTP], out_dt) for _ in range(nch)]
        axs = [[p.sb("ax", [128, NTP], aux_dts[i]) for i in range(nax)] for _ in range(nch)]
        tmps = [p.sb("tp", [128, 512], F32) for _ in range(nch)]
        pax = p.sb("pax", [128, NTP], F32) if pass_aux is not None else None
        ch = [Chain(p, f"g{i}") for i in range(nch)]
        wl = [p.sem(f"wl{i}") for i in range(nch)]
        al = [p.sem(f"al{i}") for i in range(nch)]
        cx = p.sem("cx")
        XTv = fm(XT)
        Wv = [fm(w) for w in Ws]
        T, G = nc.tensor, nc.gpsimd
        groups = [(pi, list(range(m0, min(m0 + nch, nM)))) for pi in range(npass) for m0 in range(0, nM, nch)]
        mmdone = []

        def emit_wloads(gi):
            pi, grp = groups[gi]
            for idx, mi in enumerate(grp):
                if gi >= wbuf and idx < len(mmdone[gi - wbuf]):
                    G.wait_ge(ch[idx].s.h, mmdone[gi - wbuf][idx])
                for w in range(nW):
                    wl[idx].inc(G.dma_start(out=ws[idx][gi % wbuf][w][:], in_=Wv[w][:, :, mi * 128:(mi + 1) * 128]), 16)

        emit_wloads(0)
        last_pi = -1
        for gi, (pi, grp) in enumerate(groups):
            if pi != last_pi:
                for C in ch:
                    C.wait(nc.sync)
                cx.inc(nc.sync.dma_start(out=xs[:], in_=XTv[:, :, t0 + pi * NTP: t0 + (pi + 1) * NTP]), 16)
                if pass_aux is not None:
                    cx.inc(nc.sync.dma_start(out=pax[:], in_=pass_aux(pi, NTP)), 16)
                last_pi = pi
                newpass = True
            if nax:
                for idx, mi in enumerate(grp):
                    ch[idx].wait(nc.sync)
                    for i, s_ in enumerate(auxsrcs(mi, pi, NTP)):
                        al[idx].inc(nc.sync.dma_start(out=axs[idx][i][:], in_=s_), 16)
            wl_need = [wl[idx].n for idx in range(len(grp))]
            done = [0] * len(grp)
            for sbk in range(nblk // nbs):
                for idx, mi in enumerate(grp):
                    C = ch[idx]
                    b0 = 2 * idx
                    C.wait(T)
                    if sbk == 0:
                        T.wait_ge(wl[idx].h, wl_need[idx])
                        T.wait_ge(cx.h, cx.n)
                    for w in range(nW):
                        for jj in range(nbs):
                            blk = sbk * nbs + jj
                            for kc in range(Kc):
                                ins = T.matmul(p.ps[:, b0 + w * (2 // nW) + jj, :], ws[idx][gi % wbuf][w][:, kc, :],
                                               xs[:, kc, blk * 512:(blk + 1) * 512], start=(kc == 0), stop=(kc == Kc - 1))
                    done[idx] = C.s.inc(ins)
                    for jj in range(nbs):
                        blk = sbk * nbs + jj
                        sl = slice(blk * 512, (blk + 1) * 512)
                        if sbk == 0 and jj == 0:
                            for e_ in (nc.vector, nc.scalar):
                                if pass_aux is not None:
                                    e_.wait_ge(cx.h, cx.n)
                                if nax:
                                    e_.wait_ge(al[idx].h, al[idx].n)
                        body(C, [p.ps[:, b0 + w * (2 // nW) + jj, :] for w in range(nW)], outs[idx][:, sl],
                             [a_[:, sl] for a_ in axs[idx]], tmps[idx], mi, pi, blk, (pax[:, sl] if pax is not None else None))
            mmdone.append(done)
            if gi + 1 < len(groups):
                emit_wloads(gi + 1)
            for idx, mi in enumerate(grp):
                for d in dsts(mi, pi, NTP):
                    ch[idx].dma(nc.sync, d, outs[idx][:])
        for e in (nc.sync, nc.gpsimd, nc.tensor, nc.vector, nc.scalar):
            for C in ch:
                C.wait(e)
        nc.all_engine_barrier()


def gemm_tokmajor(p, XT, K, t0, ntok, Wm, M, OUT, NTP=2048):
    nc = p.nc
    Kc = K // 128
    NTP = min(NTP, ntok)
    npass = ntok // NTP
    nch = 4
    T, V = nc.tensor, nc.vector
    with nc.cleanup_on_exit():
        xs = p.sb("xs", [128, Kc, NTP], BF16)
        wsl = p.sb("wsl", [128, Kc, 512], BF16)
        outs = [p.sb("ot", [128, 512], BF16) for _ in range(nch)]
        ch = [Chain(p, f"t{i}") for i in range(nch)]
        cx = p.sem("cx")
        XTv, Wv = fm(XT), fm(Wm)
        u = 0
        for pi in range(npass):
            for C in ch:
                C.wait(nc.sync)
            cx.inc(nc.sync.dma_start(out=xs[:], in_=XTv[:, :, t0 + pi * NTP: t0 + (pi + 1) * NTP]), 16)
            for si in range(M // 512):
                for C in ch:
                    C.wait(nc.gpsimd)
                cx.inc(nc.gpsimd.dma_start(out=wsl[:], in_=Wv[:, :, si * 512:(si + 1) * 512]), 16)
                for tb in range(NTP // 128):
                    c = u % nch
                    C = ch[c]
                    C.wait(T)
                    T.wait_ge(cx.h, cx.n)
                    for kc in range(Kc):
                        ins = T.matmul(p.ps[:, 2 * c, :], xs[:, kc, tb * 128:(tb + 1) * 128], wsl[:, kc, :],
                                       start=(kc == 0), stop=(kc == Kc - 1))
                    C.s.inc(ins)
                    C.go(V, lambda: V.tensor_copy(out=outs[c][:], in_=p.ps[:, 2 * c, :]))
                    r0 = t0 + pi * NTP + tb * 128
                    C.dma(nc.sync, OUT[r0:r0 + 128, si * 512:(si + 1) * 512], outs[c][:])
                    u += 1
        for e in (nc.sync, nc.gpsimd, T, V):
            for C in ch:
                C.wait(e)
        nc.all_engine_barrier()


def layernorm(p, RT, g_col, b_col, t0, ntok, OUT32, OUTB):
    nc, cfg = p.nc, p.cfg
    Dc = cfg.D // 128
    NL = 256
    npass = ntok // NL
    NCH = 3
    with nc.cleanup_on_exit():
        ch = [Chain(p, f"l{i}") for i in range(NCH)]
        bufs = []
        for c in range(NCH):
            bufs.append(dict(r=p.sb("lnr", [128, Dc, NL], F32), sq=p.sb("lnsq", [128, Dc, NL], F32),
                             mean=p.sb("lnm", [128, NL], F32), rstd=p.sb("lnrs", [128, NL], F32),
                             ob=p.sb("lnob", [128, Dc, NL], BF16)))
        RTv, O32v, OBv = fm(RT), fm(OUT32), fm(OUTB)
        V, A, T = nc.vector, nc.scalar, nc.tensor
        for pi in range(npass):
            c = pi % NCH
            C, B = ch[c], bufs[c]
            sl = slice(t0 + pi * NL, t0 + (pi + 1) * NL)
            r, sq, mean, rstd, ob = B["r"], B["sq"], B["mean"], B["rstd"], B["ob"]
            C.dma(nc.sync, r[:], RTv[:, :, sl])
            C.go(A, lambda: A.activation(out=sq[:], in_=r[:], func=AF.Square))
            C.wait(T)
            for kc in range(Dc):
                T.matmul(p.ps[:, 2 * c, 0:NL], p.ones32[:], r[:, kc, :], start=(kc == 0), stop=(kc == Dc - 1))
            for kc in range(Dc):
                ins = T.matmul(p.ps[:, 2 * c + 1, 0:NL], p.ones32[:], sq[:, kc, :], start=(kc == 0), stop=(kc == Dc - 1))
            C.s.inc(ins)
            C.go(V, lambda: V.tensor_scalar(out=mean[:], in0=p.ps[:, 2 * c, 0:NL], scalar1=1.0 / cfg.D, scalar2=None, op0=ALU.mult))
            C.go(V, lambda: V.tensor_tensor(out=rstd[:], in0=mean[:], in1=mean[:], op=ALU.mult))
            C.go(V, lambda: V.scalar_tensor_tensor(out=rstd[:], in0=p.ps[:, 2 * c + 1, 0:NL], scalar=1.0 / cfg.D, in1=rstd[:],
                                                   op0=ALU.mult, op1=ALU.subtract))
            C.go(A, lambda: A.activation(out=rstd[:], in_=rstd[:], func=AF.Sqrt, bias=p.epsc[:, 0:1]))
            C.go(V, lambda: V.reciprocal(out=rstd[:], in_=rstd[:]))
            C.wait(V)
            for kc in range(Dc):
                ins = V.tensor_tensor(out=sq[:, kc, :], in0=r[:, kc, :], in1=mean[:], op=ALU.subtract)
            C.s.inc(ins)
            C.wait(V)
            for kc in range(Dc):
                ins = V.tensor_tensor(out=sq[:, kc, :], in0=sq[:, kc, :], in1=rstd[:], op=ALU.mult)
            C.s.inc(ins)
            C.wait(A)
            for kc in range(Dc):
                ins = A.activation(out=r[:, kc, :], in_=sq[:, kc, :], func=AF.Identity,
                                   scale=g_col[:, kc:kc + 1], bias=b_col[:, kc:kc + 1])
            C.s.inc(ins)
            C.dma(nc.sync, O32v[:, :, sl], r[:])
            v_id = C.s.n - 16
            V.wait_ge(C.s.h, v_id)
            C.s.inc(V.tensor_copy(out=ob[:], in_=r[:]))
            C.dma(nc.sync, OBv[:, :, sl], ob[:])
        for e in (nc.sync, nc.gpsimd, T, V, A):
            for C in ch:
                C.wait(e)
        nc.all_engine_barrier()


def pool_stage(p, UT32, t0, ntok, ZT):
    nc, cfg = p.nc, p.cfg
    npass = ntok // 512
    nch = cfg.PW // 128
    W = 528
    with nc.cleanup_on_exit():
        ch = [Chain(p, "pa"), Chain(p, "pb")]
        bufs = [dict(u=p.sb("pu", [128, W], F32), a=p.sb("pa", [128, W], F32), b=p.sb("pb", [128, W], F32),
                     val=p.sb("pv", [128, W], F32), ic=p.sb("pi", [128, 512], F32), z=p.sb("pz", [128, 512], BF16))
                for _ in range(2)]
        V = nc.vector
        Uv = fm(UT32)
        Zv = fm(ZT)
        un = 0
        for pi in range(npass):
            for ci in range(nch):
                g = ci // (cfg.PG // 128)
                w = 2 << g
                c = un % 2
                C, B = ch[c], bufs[c]
                u, sa, sbb, val, ic, z = B["u"], B["a"], B["b"], B["val"], B["ic"], B["z"]
                a0 = t0 + pi * 512
                if a0 == 0:
                    C.go(V, lambda: V.memset(u[:, 0:16], 0.0))
                    C.go(V, lambda: V.memset(val[:, 0:16], 0.0))
                    C.dma(nc.sync, u[:, 16:W], Uv[:, ci, 0:512])
                    C.dma(nc.sync, val[:, 16:W], bcast_row(p.tokvalid[0:1, 0:512], 512))
                else:
                    C.dma(nc.sync, u[:], Uv[:, ci, a0 - 16:a0 + 512])
                    C.dma(nc.sync, val[:], bcast_row(p.tokvalid[0:1, a0 - 16:a0 + 512], W))
                C.dma(nc.sync, ic[:], bcast_row(p.invcnt[g:g + 1, a0:a0 + 512], 512))
                C.go(V, lambda: V.tensor_tensor(out=u[:], in0=u[:], in1=val[:], op=ALU.mult))
                src = u
                lo = 0
                step = 1
                dsts = [sa, sbb]
                k = 0
                while step < w:
                    d = dsts[k % 2]
                    C.go(V, lambda: V.tensor_tensor(out=d[:, lo + step:W], in0=src[:, lo + step:W],
                                                    in1=src[:, lo:W - step], op=ALU.add))
                    src = d
                    lo += step
                    step *= 2
                    k += 1
                d = dsts[k % 2]
                C.go(V, lambda: V.tensor_tensor(out=d[:, 16:W], in0=src[:, 16:W], in1=ic[:], op=ALU.mult))
                C.go(V, lambda: V.tensor_tensor(out=z[:], in0=d[:, 16:W], in1=u[:, 16:W], op=ALU.subtract))
                C.dma(nc.sync, Zv[:, ci, a0:a0 + 512], z[:])
                un += 1
        for e in (nc.sync, V):
            ch[0].wait(e)
            ch[1].wait(e)
        nc.all_engine_barrier()


def attention(p, l, QT, KT, V_tm, t0, ntok, AOT, lam_col, sub_col):
    nc, cfg = p.nc, p.cfg
    P = cfg.P
    nkb = P // 128
    T, V, A = nc.tensor, nc.vector, nc.scalar
    NX = 3
    with nc.cleanup_on_exit():
        kt = p.sb("kt", [128, P], BF16)
        vt = p.sb("vt", [128, nkb, 128], BF16)
        qt = p.sb("qt", [128, ntok], BF16)
        btp = p.sb("btp", [128, 1024], BF16)
        pt = [p.sb("pt", [128, 2, 512], BF16) for _ in range(NX)]
        zacc = [p.sb("zacc", [128, 2, 512], F32) for _ in range(NX)]
        rz = [p.sb("rz", [128, 512], F32) for _ in range(2)]
        o = p.sb("o", [128, 512], F32)
        o1 = p.sb("o1", [128, 512], F32)
        sq = p.sb("osq", [128, 512], F32)
        ao = p.sb("ao", [128, 512], BF16)
        ch = [Chain(p, f"c{i}") for i in range(NX)]
        zs = [p.sem(f"zs{i}") for i in range(NX)]
        E = Chain(p, "ep")
        hx = p.sem("hx")
        for h in range(cfg.H):
            for C in ch + [E]:
                C.wait(nc.sync)
            hx.inc(nc.sync.dma_start(out=kt[:], in_=KT[h * 128:(h + 1) * 128, :]), 16)
            hx.inc(nc.sync.dma_start(out=vt[:], in_=V_tm[:, h * 128:(h + 1) * 128].rearrange("(kb k) e -> k kb e", k=128)), 16)
            hx.inc(nc.sync.dma_start(out=qt[:], in_=QT[h * 128:(h + 1) * 128, t0:t0 + ntok]), 16)
            hx.inc(nc.sync.dma_start(out=btp[:], in_=p.BTP[h]), 16)
            for qi in range(ntok // 512):
                gq = (t0 // 512) + qi
                nk = 4 * gq + 4

                def emit_scores(kb):
                    jr = kb - 4 * gq
                    near = jr >= -1
                    x = kb % NX
                    C = ch[x]
                    C.wait(T)
                    T.wait_ge(hx.h, hx.n)
                    if kb < NX:
                        E.wait(T)
                    for c in range(2):
                        ins = T.matmul(p.ps[:, 2 * x + c, :], kt[64 * c:64 * c + 64, kb * 128:(kb + 1) * 128],
                                       qt[64 * c:64 * c + 64, qi * 512:(qi + 1) * 512], start=True, stop=not near)
                        if near:
                            off = 384 - 128 * jr
                            ins = T.matmul(p.ps[:, 2 * x + c, :], p.Jm[:], btp[:, off:off + 512], start=False, stop=True)
                    C.s.inc(ins)

                emit_scores(0)
                emit_scores(1)
                for kb in range(nk):
                    jr = kb - 4 * gq
                    near = jr >= -1
                    x = kb % NX
                    C = ch[x]
                    if kb + 2 < nk:
                        emit_scores(kb + 2)
                    bias = p.kvb[:, kb:kb + 1] if near else p.farb[:, h * nkb + kb:h * nkb + kb + 1]
                    if zs[x].n:
                        A.wait_ge(zs[x].h, zs[x].n)
                    C.go(A, lambda: A.activation(out=pt[x][:], in_=p.ps[:, 2 * x:2 * x + 2, :], func=AF.Exp, scale=0.125, bias=bias))
                    v_exp = C.s.n
                    V.wait_ge(C.s.h, v_exp)
                    if kb < NX:
                        E.wait(V)
                        zs[x].inc(V.tensor_copy(out=zacc[x][:], in_=pt[x][:]))
                    else:
                        V.wait_ge(zs[x].h, zs[x].n)
                        zs[x].inc(V.tensor_tensor(out=zacc[x][:], in0=zacc[x][:], in1=pt[x][:], op=ALU.add))
                    C.wait(T)
                    if kb == 0:
                        E.wait(T)
                    for c in range(2):
                        ins = T.matmul(p.ps[:, 6 + c, :], vt[:, kb, :], pt[x][:, c, :], start=(kb == 0), stop=(kb == nk - 1))
                    C.s.inc(ins)
                for C in ch:
                    C.wait(V)
                for x in range(NX):
                    V.wait_ge(zs[x].h, zs[x].n)
                E.go(V, lambda: V.tensor_tensor(out=zacc[0][:], in0=zacc[0][:], in1=zacc[1][:], op=ALU.add))
                E.go(V, lambda: V.tensor_tensor(out=zacc[0][:], in0=zacc[0][:], in1=zacc[2][:], op=ALU.add))
                E.wait(T)
                for C in ch:
                    C.wait(T)
                T.matmul(p.ps[:, 0, :], p.ones32[:], zacc[0][:, 0, :], start=True, stop=True)
                ins = T.matmul(p.ps[:, 1, :], p.ones32[:], zacc[0][:, 1, :], start=True, stop=True)
                E.s.inc(ins)
                for c in range(2):
                    E.go(V, lambda: V.tensor_scalar(out=rz[c][:], in0=p.ps[:, c, :], scalar1=1e-30, scalar2=None, op0=ALU.add))
                    E.go(V, lambda: V.reciprocal(out=rz[c][:], in_=rz[c][:]))
                E.go(V, lambda: V.tensor_tensor(out=o[:], in0=p.ps[:, 6, :], in1=rz[0][:], op=ALU.mult))
                E.go(V, lambda: V.tensor_tensor(out=o1[:], in0=p.ps[:, 7, :], in1=rz[1][:], op=ALU.mult))
                E.go(V, lambda: V.scalar_tensor_tensor(out=o[:], in0=o1[:], scalar=lam_col[:, 0:1], in1=o[:], op0=ALU.mult, op1=ALU.add))
                E.go(V, lambda: V.tensor_tensor(out=sq[:], in0=o[:], in1=o[:], op=ALU.mult))
                E.go(T, lambda: T.matmul(p.ps[:, 2, :], p.ones32[:], sq[:], start=True, stop=True))
                E.go(V, lambda: V.tensor_scalar(out=sq[:], in0=p.ps[:, 2, :], scalar1=1.0 / 128, scalar2=None, op0=ALU.mult))
                E.go(A, lambda: A.activation(out=sq[:], in_=sq[:], func=AF.Sqrt, bias=p.epsc[:, 0:1]))
                E.go(V, lambda: V.reciprocal(out=sq[:], in_=sq[:]))
                E.go(V, lambda: V.tensor_tensor(out=o[:], in0=o[:], in1=sq[:], op=ALU.mult))
                E.go(V, lambda: V.tensor_scalar(out=ao[:], in0=o[:], scalar1=sub_col[:, 0:1], scalar2=None, op0=ALU.mult))
                E.dma(nc.sync, AOT[h * 128:(h + 1) * 128, t0 + qi * 512:t0 + (qi + 1) * 512], ao[:])
        for e in (nc.sync, T, V, A):
            for C in ch + [E]:
                C.wait(e)
            for x in range(NX):
                e.wait_ge(zs[x].h, zs[x].n)
        nc.all_engine_barrier()


def setup_consts(p, l_count):
    nc, cfg = p.nc, p.cfg
    P, H = cfg.P, cfg.H
    nkb = P // 128
    Dc = cfg.D // 128
    V, A, T = nc.vector, nc.scalar, nc.tensor
    C = Chain(p, "su")
    p.ones32 = p.sb("ones32", [128, 128], F32)
    p.onesb = p.sb("onesb", [128, 128], BF16)
    p.Jm = p.sb("Jm", [128, 128], BF16)
    p.kvb = p.sb("kvb", [128, nkb], F32)
    p.farb = p.sb("farb", [128, H * nkb], F32)
    p.lncols = {}
    p.epsc = p.sb("epsc", [128, 1], F32)
    C.go(V, lambda: V.memset(p.epsc[:], 1e-5))
    C.go(V, lambda: V.memset(p.ones32[:], 1.0))
    C.go(V, lambda: V.memset(p.onesb[:], 1.0))
    C.dma(nc.gpsimd, p.Jm[:], p.J_in[:, :])
    C.dma(nc.sync, p.kvb[:], bcast_row(p.kvalid[0:1, :], nkb))
    for l in range(l_count):
        for nm, src in (("g1", p.w["ln1_g"]), ("b1", p.w["ln1_b"]), ("g2", p.w["ln2_g"]), ("b2", p.w["ln2_b"])):
            t = p.sb(f"{nm}{l}", [128, Dc], F32)
            C.dma(nc.sync, t[:], src[l].rearrange("(c q) -> q c", q=128), allow_slow_non_contiguous=True)
            p.lncols[(nm, l)] = t
        t = p.sb(f"psc{l}", [128, cfg.PW // 128], F32)
        C.dma(nc.sync, t[:], p.w["pool_scale"][l].rearrange("(c q) -> q c", q=128), allow_slow_non_contiguous=True)
        p.lncols[("psc", l)] = t
        li = 0.8 - 0.6 * math.exp(-0.3 * l)
        t = p.sb(f"sub{l}", [128, 1], F32)
        C.dma(nc.sync, t[:], p.w["subln_w"][l].rearrange("(q o) -> q o", o=1), allow_slow_non_contiguous=True)
        C.go(V, lambda: V.tensor_scalar(out=t[:], in0=t[:], scalar1=1.0 - li, scalar2=None, op0=ALU.mult))
        p.lncols[("sub", l)] = t
        lv = p.sb(f"lv{l}", [128, 4, 64], F32)
        C.dma(nc.sync, lv[:], bass.AP(p.w["lambdas"][l].tensor, p.w["lambdas"][l].offset, [[0, 128], [64, 4], [1, 64]]))
        pr = p.sb(f"lpr{l}", [128, 2, 64], F32)
        C.go(V, lambda: V.tensor_tensor(out=pr[:, 0, :], in0=lv[:, 0, :], in1=lv[:, 1, :], op=ALU.mult))
        C.go(V, lambda: V.tensor_tensor(out=pr[:, 1, :], in0=lv[:, 2, :], in1=lv[:, 3, :], op=ALU.mult))
        sm = p.sb(f"lsm{l}", [128, 2], F32)
        C.go(V, lambda: V.tensor_reduce(out=sm[:], in_=pr[:], axis=AX.X, op=ALU.add))
        C.go(A, lambda: A.activation(out=sm[:], in_=sm[:], func=AF.Exp))
        lam = p.sb(f"lam{l}", [128, 1], F32)
        C.go(V, lambda: V.tensor_tensor(out=lam[:], in0=sm[:, 1:2], in1=sm[:, 0:1], op=ALU.subtract))
        C.go(V, lambda: V.tensor_scalar(out=lam[:], in0=lam[:], scalar1=-li, scalar2=None, op0=ALU.add))
        p.lncols[("nlam", l)] = lam
    rb31 = p.sb("rb31", [128, H], F32)
    C.dma(nc.sync, rb31[:], bcast_row(p.w["rel_bias"][31:32, :], H))
    for h in range(H):
        C.go(V, lambda: V.tensor_scalar(out=p.farb[:, h * nkb:(h + 1) * nkb], in0=p.kvb[:], scalar1=rb31[:, h:h + 1],
                                        scalar2=None, op0=ALU.add))
    rba = p.sb("rba", [33, H], F32)
    oh = p.sb("oh", [33, cfg.GW], F32)
    gsb = p.sb("gsb", [H, cfg.GW], BF16)
    C.go(V, lambda: V.memset(rba[:], 1.0))
    C.dma(nc.sync, rba[0:32, :], p.w["rel_bias"][:, :])
    C.dma(nc.sync, oh[:], p.OH_in[:, :])
    for j in range(0, cfg.GW, 384):
        C.go(T, lambda: T.matmul(p.ps[0:H, 0, 0:384], rba[:], oh[:, j:j + 384], start=True, stop=True))
        C.go(V, lambda: V.tensor_scalar(out=gsb[:, j:j + 384], in0=p.ps[0:H, 0, 0:384], scalar1=8.0, scalar2=None, op0=ALU.mult))
    C.dma(nc.sync, p.Gd[:, :], gsb[:])
    C.wait(nc.gpsimd)
    C.wait(nc.scalar)
    bts = p.sem("bts")
    engs = [nc.sync, nc.gpsimd, nc.scalar]
    i = 0
    for h in range(H):
        for k in range(128):
            e = engs[i % 3]
            bts.inc(e.dma_start(out=p.BTP[h, k:k + 1, :], in_=p.Gd[h:h + 1, k:k + 1024]), 16)
            i += 1
    for e in (nc.sync, nc.gpsimd, T, V, A):
        C.wait(e)
        e.wait_ge(bts.h, bts.n)
    nc.all_engine_barrier()


def router_stage(p, l, H32, t0, ntok, GT):
    nc, cfg = p.nc, p.cfg
    Dc = cfg.D // 128
    NE = cfg.NE
    V, A, T = nc.vector, nc.scalar, nc.tensor
    with nc.cleanup_on_exit():
        C = Chain(p, "rt")
        rw = p.sb("rw", [128, Dc, NE], F32)
        hs = p.sb("hs", [128, Dc, 512], F32)
        lg = p.sb("lg", [128, NE], F32)
        l2 = p.sb("l2", [128, NE], F32)
        e1 = p.sb("e1", [128, NE], F32)
        e2 = p.sb("e2", [128, NE], F32)
        m1 = p.sb("m1", [128, 1], F32)
        m2 = p.sb("m2", [128, 1], F32)
        g1 = p.sb("g1", [128, 1], F32)
        g2 = p.sb("g2", [128, 1], F32)
        C.dma(nc.sync, rw[:], p.w["router_w"][l // 2].rearrange("(c q) e -> q c e", q=128))
        Hv = fm(H32)
        for pi in range(ntok // 512):
            C.dma(nc.sync, hs[:], Hv[:, :, t0 + pi * 512:t0 + (pi + 1) * 512])
            for tb in range(4):
                C.wait(T)
                for kc in range(Dc):
                    ins = T.matmul(p.ps[:, 0, 0:NE], hs[:, kc, tb * 128:(tb + 1) * 128], rw[:, kc, :],
                                   start=(kc == 0), stop=(kc == Dc - 1))
                C.s.inc(ins)
                C.go(V, lambda: V.tensor_copy(out=lg[:], in_=p.ps[:, 0, 0:NE]))
                C.go(V, lambda: V.tensor_reduce(out=m1[:], in_=lg[:], axis=AX.X, op=ALU.max))
                C.go(V, lambda: V.tensor_scalar(out=e1[:], in0=lg[:], scalar1=m1[:, 0:1], scalar2=None, op0=ALU.is_equal))
                C.go(V, lambda: V.scalar_tensor_tensor(out=l2[:], in0=e1[:], scalar=-1e30, in1=lg[:], op0=ALU.mult, op1=ALU.add))
                C.go(V, lambda: V.tensor_reduce(out=m2[:], in_=l2[:], axis=AX.X, op=ALU.max))
                C.go(V, lambda: V.tensor_scalar(out=e2[:], in0=l2[:], scalar1=m2[:, 0:1], scalar2=None, op0=ALU.is_equal))
                C.go(V, lambda: V.tensor_tensor(out=g2[:], in0=m1[:], in1=m2[:], op=ALU.subtract))
                C.go(A, lambda: A.activation(out=g1[:], in_=g2[:], func=AF.Sigmoid))
                C.go(V, lambda: V.tensor_scalar(out=g2[:], in0=g1[:], scalar1=-1.0, scalar2=1.0, op0=ALU.mult, op1=ALU.add))
                C.go(V, lambda: V.tensor_scalar(out=e1[:], in0=e1[:], scalar1=g1[:, 0:1], scalar2=None, op0=ALU.mult))
                C.go(V, lambda: V.scalar_tensor_tensor(out=e1[:], in0=e2[:], scalar=g2[:, 0:1], in1=e1[:], op0=ALU.mult, op1=ALU.add))
                a0 = pi * 512 + tb * 128
                C.dma(nc.sync, GT[:, a0:a0 + 128].rearrange("e t -> t e"), e1[:], allow_slow_non_contiguous=True)
        for e in (nc.sync, T, V, A):
            C.wait(e)
        nc.all_engine_barrier()


def build(cfg):
    p = Prog(cfg)
    nc = p.nc
    D, P, OWN, H, AW, PW = cfg.D, cfg.P, cfg.OWN, cfg.H, cfg.AW, cfg.PW
    L = cfg.DEPTH
    NDn, NMo = (L + 1) // 2, L // 2
    w = {}
    w["xT"] = p.din("xT", [D, P])
    w["w_in"] = p.din("w_in", [L, D, cfg.INW])
    w["lambdas"] = p.din("lambdas", [L, 4, 64])
    w["subln_w"] = p.din("subln_w", [L, 128])
    w["pool_w"] = p.din("pool_w", [L, 4, cfg.PG, cfg.PG])
    w["pool_scale"] = p.din("pool_scale", [L, PW])
    w["w_branch_attn"] = p.din("w_branch_attn", [L, AW, D])
    w["w_branch_pool"] = p.din("w_branch_pool", [L, PW, D])
    w["w_out"] = p.din("w_out", [L, D, D])
    w["rel_bias"] = p.din("rel_bias", [32, H])
    for nm in ("ln1_g", "ln1_b", "ln2_g", "ln2_b"):
        w[nm] = p.din(nm, [L, D])
    w["dense_w_gate"] = p.din("dense_w_gate", [NDn, D, cfg.DFF])
    w["dense_w_up"] = p.din("dense_w_up", [NDn, D, cfg.DFF])
    w["dense_w_down"] = p.din("dense_w_down", [NDn, cfg.DFF, D])
    w["router_w"] = p.din("router_w", [NMo, D, cfg.NE])
    w["moe_w_gate"] = p.din("moe_w_gate", [NMo, cfg.NE, D, cfg.DFE])
    w["moe_w_up"] = p.din("moe_w_up", [NMo, cfg.NE, D, cfg.DFE])
    w["moe_w_down"] = p.din("moe_w_down", [NMo, cfg.NE, cfg.DFE, D])
    p.w = w
    p.kvalid = p.din("kvalid", [1, P // 128])
    p.tokvalid = p.din("tokvalid", [1, P])
    p.invcnt = p.din("invcnt", [4, P])
    p.OH_in = p.din("OH", [33, cfg.GW])
    p.J_in = p.din("Jm", [128, 128])
    yT = nc.dram_tensor("yT", [D, OWN], F32, kind="ExternalOutput").ap()
    p.Gd = p.dram("Gd", [H, cfg.GW], BF16)
    p.BTP = p.dram("BTP", [H, 128, 1024], BF16)
    XB = p.dram("XB", [D, P], BF16)
    QT = p.dram("QT", [AW, P], BF16)
    KT = p.dram("KT", [AW, P], BF16)
    Vt = p.dram("Vt", [P, AW], BF16)
    UT = p.dram("UT", [PW, P], F32)
    SGA = p.dram("SGA", [D, P], BF16)
    SGP = p.dram("SGP", [D, P], BF16)
    AOT = p.dram("AOT", [AW, P], BF16)
    ZT = p.dram("ZT", [PW, P], BF16)
    POT = p.dram("POT", [PW, P], BF16)
    MT = p.dram("MT", [D, P], BF16)
    RT = p.dram("RT", [D, P], F32)
    H32 = p.dram("H32", [D, P], F32)
    HB = p.dram("HB", [D, P], BF16)
    X32 = p.dram("X32", [D, P], F32)
    HE = p.dram("HE", [max(cfg.DFF, cfg.DFE), P], BF16)
    GT = p.dram("GT", [cfg.NE, OWN], F32)
    FA = p.dram("FA", [D, OWN], F32)
    p.ps = nc.alloc_psum_tensor("ps", [128, 8, 512], F32)
    V, A, T = nc.vector, nc.scalar, nc.tensor

    setup_consts(p, L)

    with nc.cleanup_on_exit():
        C = Chain(p, "xc")
        step = min(D, max(128, (1 << 22) // (P * 4) // 128 * 128))
        for r0 in range(0, D, step):
            C.dma(nc.gpsimd, XB[r0:r0 + step, :], w["xT"][r0:r0 + step, :])
        C.wait(nc.gpsimd)
        nc.all_engine_barrier()

    Xin32 = w["xT"]
    for l in range(L):
        t0 = 0 if l == 0 else P - OWN
        ntok = P - t0
        wi = w["w_in"][l]
        o_q, o_k, o_v, o_u, o_ga, o_gp = 0, AW, 2 * AW, 3 * AW, 3 * AW + PW, 3 * AW + PW + D
        NTq = min(2048, ntok)

        def ecopy(C, ps, out, aux, tmp, *a):
            C.go(V, lambda: V.tensor_copy(out=out, in_=ps[0]))

        def esig(C, ps, out, aux, tmp, *a):
            C.go(A, lambda: A.activation(out=out, in_=ps[0], func=AF.Sigmoid))

        def dcols(DST, toff):
            return lambda mi, pi, n: [DST[mi * 128:(mi + 1) * 128, toff + pi * n:toff + (pi + 1) * n]]

        gemm2(p, XB, D, t0, ntok, [wi[:, o_q:o_q + AW]], AW, ecopy, dcols(QT, t0), 2048)
        gemm2(p, XB, D, 0, P, [wi[:, o_k:o_k + AW]], AW, ecopy, dcols(KT, 0), 2048)
        gemm_tokmajor(p, XB, D, 0, P, wi[:, o_v:o_v + AW], AW, Vt)
        tu = max(0, t0 - 512)
        gemm2(p, XB, D, tu, P - tu, [wi[:, o_u:o_u + PW]], PW, ecopy, dcols(UT, tu), (2048 if (P - tu) % 2048 == 0 else 512), out_dt=F32)
        gemm2(p, XB, D, t0, ntok, [wi[:, o_ga:o_ga + D]], D, esig, dcols(SGA, t0), 2048)
        gemm2(p, XB, D, t0, ntok, [wi[:, o_gp:o_gp + D]], D, esig, dcols(SGP, t0), 2048)
        attention(p, l, QT, KT, Vt, t0, ntok, AOT, p.lncols[("nlam", l)], p.lncols[("sub", l)])
        pool_stage(p, UT, t0, ntok, ZT)
        psc = p.lncols[("psc", l)]
        for g in range(4):
            PG = cfg.PG

            def epool(C, ps, out, aux, tmp, mi, pi, blk, pax, g=g):
                col = g * (PG // 128) + mi
                C.go(V, lambda: V.tensor_scalar(out=out, in0=ps[0], scalar1=psc[:, col:col + 1], scalar2=None, op0=ALU.mult))
            gemm2(p, ZT[g * PG:(g + 1) * PG, :], PG, t0, ntok, [w["pool_w"][l, g]], PG, epool,
                  (lambda mi, pi, n, g=g: [POT[g * PG + mi * 128:g * PG + (mi + 1) * 128, t0 + pi * n:t0 + (pi + 1) * n]]), 2048)
        merge_gemm(p, AOT, AW, t0, ntok, w["w_branch_attn"][l], SGA, None, MT)
        merge_gemm(p, POT, PW, t0, ntok, w["w_branch_pool"][l], SGP, MT, MT)
        resid_gemm(p, MT, D, t0, ntok, [w["w_out"][l]], Xin32, RT, cfg.ALPHA, 0)
        layernorm(p, RT, p.lncols[("g1", l)], p.lncols[("b1", l)], t0, ntok, H32, HB)
        if l % 2 == 0:
            DFF = cfg.DFF

            def eswi(C, ps, out, aux, tmp, *a):
                C.go(A, lambda: A.activation(out=tmp[:], in_=ps[0], func=AF.Silu))
                C.go(V, lambda: V.tensor_tensor(out=out, in0=tmp[:], in1=ps[1], op=ALU.mult))
            gemm2(p, HB, D, t0, ntok, [w["dense_w_gate"][l // 2], w["dense_w_up"][l // 2]], DFF, eswi, dcols(HE, t0), 2048)
            resid_gemm(p, HE[0:DFF, :], DFF, t0, ntok, [w["dense_w_down"][l // 2]], H32, RT, cfg.ALPHA, 0, NTP=1024)
        else:
            router_stage(p, l, H32, t0, ntok, GT)
            for e in range(cfg.NE):
                moe_up_gemm(p, HB, t0, ntok, w["moe_w_gate"][l // 2, e], w["moe_w_up"][l // 2, e], GT, e, HE)
                resid_gemm(p, HE[0:cfg.DFE, :], cfg.DFE, t0, ntok, [w["moe_w_down"][l // 2, e]],
                           H32 if e == 0 else None, FA, cfg.ALPHA, t0, acc_prev=(e > 0), final_dst=(RT if e == cfg.NE - 1 else None), NTP=1024)
        if l == L - 1:
            layernorm(p, RT, p.lncols[("g2", l)], p.lncols[("b2", l)], t0, ntok, X32, XB)
            with nc.cleanup_on_exit():
                C = Chain(p, "oc")
                for r0 in range(0, D, 128):
                    C.dma(nc.sync, yT[r0:r0 + 128, :], X32[r0:r0 + 128, P - OWN:P])
                C.wait(nc.sync)
                nc.all_engine_barrier()
        else:
            layernorm(p, RT, p.lncols[("g2", l)], p.lncols[("b2", l)], t0, ntok, X32, XB)
            Xin32 = X32
    return p


def merge_gemm(p, XT, K, t0, ntok, Wm, SG, PREV, OUT):
    V = p.nc.vector
    D = p.cfg.D

    def srcs(mi, pi, n):
        s = [SG[mi * 128:(mi + 1) * 128, t0 + pi * n:t0 + (pi + 1) * n]]
        if PREV is not None:
            s.append(PREV[mi * 128:(mi + 1) * 128, t0 + pi * n:t0 + (pi + 1) * n])
        return s

    def body(C, ps, out, ax, tmp, mi, pi, blk, pax):
        if PREV is None:
            C.go(V, lambda: V.tensor_tensor(out=out, in0=ps[0], in1=ax[0], op=ALU.mult))
        else:
            C.go(V, lambda: V.tensor_tensor(out=tmp[:], in0=ps[0], in1=ax[0], op=ALU.mult))
            C.go(V, lambda: V.tensor_tensor(out=out, in0=tmp[:], in1=ax[1], op=ALU.add))
    gemm2(p, XT, K, t0, ntok, [Wm], D, body,
          lambda mi, pi, n: [OUT[mi * 128:(mi + 1) * 128, t0 + pi * n:t0 + (pi + 1) * n]], 2048, out_dt=BF16,
          auxsrcs=srcs, aux_dts=([BF16] if PREV is None else [BF16, BF16]))


def resid_gemm(p, XT, K, t0, ntok, Ws, RES32, OUT, alpha, o0, acc_prev=False, final_dst=None, NTP=None):
    V = p.nc.vector
    D = p.cfg.D
    if NTP is None:
        NTP = 1024 if K // 128 <= 16 else 512

    def srcs(mi, pi, n):
        s = []
        if RES32 is not None:
            s.append(RES32[mi * 128:(mi + 1) * 128, t0 + pi * n:t0 + (pi + 1) * n])
        if acc_prev:
            s.append(OUT[mi * 128:(mi + 1) * 128, t0 - o0 + pi * n:t0 - o0 + (pi + 1) * n])
        return s

    def body(C, ps, out, ax, tmp, mi, pi, blk, pax):
        i = 0
        cur = ps[0]
        if RES32 is not None:
            C.go(V, lambda: V.scalar_tensor_tensor(out=out, in0=ax[0], scalar=alpha, in1=cur, op0=ALU.mult, op1=ALU.add))
            cur = out
            i = 1
        if acc_prev:
            cc = cur
            C.go(V, lambda: V.tensor_tensor(out=out, in0=cc, in1=ax[i], op=ALU.add))
            cur = out
        if cur is ps[0]:
            C.go(V, lambda: V.tensor_copy(out=out, in_=ps[0]))

    def dst(mi, pi, n):
        d = [OUT[mi * 128:(mi + 1) * 128, t0 - o0 + pi * n:t0 - o0 + (pi + 1) * n]]
        if final_dst is not None:
            d.append(final_dst[mi * 128:(mi + 1) * 128, t0 + pi * n:t0 + (pi + 1) * n])
        return d
    nax = (RES32 is not None) + (1 if acc_prev else 0)
    gemm2(p, XT, K, t0, ntok, [Ws[0]], D, body, dst, NTP, out_dt=F32, auxsrcs=srcs, aux_dts=[F32] * nax)


def moe_up_gemm(p, HB, t0, ntok, Wg, Wu, GT, e, HE):
    nc, cfg = p.nc, p.cfg
    V, A = nc.vector, nc.scalar

    def body(C, ps, out, ax, tmp, mi, pi, blk, pax):
        C.go(A, lambda: A.activation(out=tmp[:], in_=ps[0], func=AF.Silu))
        C.go(V, lambda: V.tensor_tensor(out=tmp[:], in0=tmp[:], in1=ps[1], op=ALU.mult))
        C.go(V, lambda: V.tensor_tensor(out=out, in0=tmp[:], in1=pax, op=ALU.mult))
    gemm2(p, HB, cfg.D, t0, ntok, [Wg, Wu], cfg.DFE, body,
          lambda mi, pi, n: [HE[mi * 128:(mi + 1) * 128, t0 + pi * n:t0 + (pi + 1) * n]], 2048, out_dt=BF16,
          pass_aux=lambda pi, n: bcast_row(GT[e:e + 1, pi * n:(pi + 1) * n], n))


def t5_bucket_np(n):
    n = np.maximum(n, 0)
    large = 16 + (np.log(np.maximum(n, 1).astype(np.float32) / 16) / math.log(128 / 16) * 16).astype(np.int32)
    large = np.minimum(large, 31)
    return np.where(n < 16, n, large)


def host_consts(cfg, r):
    P, OWN = cfg.P, cfg.OWN
    nreal = OWN * (r + 1)
    pad = P - nreal
    kvalid = np.zeros((1, P // 128), np.float32)
    kvalid[0, :pad // 128] = NEG
    tokvalid = np.zeros((1, P), np.float32)
    tokvalid[0, pad:] = 1.0
    invcnt = np.zeros((4, P), np.float32)
    t = np.arange(nreal)
    for g, wdw in enumerate((2, 4, 8, 16)):
        invcnt[g, pad:] = 1.0 / np.minimum(t + 1, wdw).astype(np.float32)
    n = np.arange(cfg.GW) - 511
    OH = np.zeros((33, cfg.GW), np.float32)
    b = t5_bucket_np(n)
    ok = n >= 0
    OH[b[ok], np.nonzero(ok)[0]] = 1.0
    OH[32, ~ok] = NEG
    J = np.zeros((128, 128), np.float32)
    J[np.arange(128), 127 - np.arange(128)] = 1.0
    return dict(kvalid=kvalid, tokvalid=tokvalid, invcnt=invcnt, OH=OH, Jm=J)


_CACHE = {}


def run(cfg, inputs):
    key = id(cfg)
    if key not in _CACHE:
        _CACHE[key] = build(cfg)
    p = _CACHE[key]
    x = np.asarray(inputs["x"], np.float32)
    ncore = 4 * cfg.BATCH
    in_maps = []
    for c in range(ncore):
        b, r = divmod(c, 4)
        nreal = cfg.OWN * (r + 1)
        xp = np.zeros((cfg.P, cfg.D), np.float32)
        xp[cfg.P - nreal:] = x[b, :nreal]
        m = {"xT": np.ascontiguousarray(xp.T)}
        for k, v in inputs.items():
            if k != "x":
                m[k] = np.asarray(v, np.float32)
        m.update(host_consts(cfg, r))
        in_maps.append(m)
    res = run_bass_kernel_spmd(p.nc, in_maps, core_ids=list(range(ncore)))
    out = np.zeros((cfg.BATCH, 4 * cfg.OWN, cfg.D), np.float32)
    for c in range(ncore):
        b, r = divmod(c, 4)
        out[b, r * cfg.OWN:(r + 1) * cfg.OWN] = res.results[c]["yT"].T
    return out


FULL = Cfg()


def kernel(**inputs):
    return run(FULL, inputs)
```

```python
import math
import numpy as np
import concourse.bass as bass
import concourse.mybir as mybir
from concourse.bass_utils import run_bass_kernel_spmd

F32, BF16 = mybir.dt.float32, mybir.dt.bfloat16
AF = mybir.ActivationFunctionType
ALU = mybir.AluOpType
AX = mybir.AxisListType
NEG = -30000.0


class Cfg:
    def __init__(s, D=2048, P=8192, OWN=2048, H=8, PG=256, DFF=5632, NE=8, DFE=7168, DEPTH=2, BATCH=2):
        s.D, s.P, s.OWN, s.H, s.PG, s.DFF, s.NE, s.DFE, s.DEPTH, s.BATCH = D, P, OWN, H, PG, DFF, NE, DFE, DEPTH, BATCH
        s.AW = H * 128
        s.PW = 4 * PG
        s.INW = 3 * s.AW + s.PW + 2 * D
        s.ALPHA = (2.0 * DEPTH) ** 0.25
        s.GW = 1152


class Sem:
    def __init__(s, nc, name):
        s.h = nc.alloc_semaphore(name=name)
        s.n = 0

    def inc(s, ins, k=1):
        ins.then_inc(s.h, k)
        s.n += k
        return s.n


class Chain:
    def __init__(s, p, name):
        s.s = p.sem(name)

    def go(s, eng, fn, k=1, extra=()):
        if s.s.n:
            eng.wait_ge(s.s.h, s.s.n)
        for (sm, v) in extra:
            if v:
                eng.wait_ge(sm.h, v)
        ins = fn()
        s.s.inc(ins, k)
        return ins

    def dma(s, eng, out, in_, extra=(), **kw):
        return s.go(eng, lambda: eng.dma_start(out=out, in_=in_, **kw), 16, extra)

    def wait(s, eng):
        if s.s.n:
            eng.wait_ge(s.s.h, s.s.n)


class Prog:
    def __init__(p, cfg):
        p.cfg = cfg
        p.nc = bass.Bass("TRN2", target_bir_lowering=False)
        p.uid = 0

    def din(p, name, shape, dt=F32):
        return p.nc.dram_tensor(name, list(shape), dt, kind="ExternalInput").ap()

    def dram(p, name, shape, dt):
        return p.nc.dram_tensor(name, list(shape), dt).ap()

    def sb(p, name, shape, dt):
        p.uid += 1
        return p.nc.alloc_sbuf_tensor(f"{name}_{p.uid}", list(shape), dt)

    def sem(p, name):
        p.uid += 1
        return Sem(p.nc, f"{name}_{p.uid}")


def fm(ap):
    return ap.rearrange("(kc q) n -> q kc n", q=128)


def bcast_row(ap_row, n):
    return bass.AP(ap_row.tensor, ap_row.offset, [[0, 128], [1, n]])


def gemm(p, XT, K, t0, ntok, Ws, M, epi, dst, NTP, out_dt=BF16, tokmajor=False):
    nc = p.nc
    Kc = K // 128
    nW = len(Ws)
    nb = NTP // 512
    assert nW * nb <= 4 and ntok % NTP == 0
    npass = ntok // NTP
    with nc.cleanup_on_exit():
        xs = p.sb("xs", [128, Kc, NTP], BF16)
        MWd = 512 if tokmajor else 128
        ws = [[p.sb("ws", [128, Kc, MWd], BF16) for _ in range(nW)] for _ in range(2)]
        outs = [p.sb("ot", [128, (512 if tokmajor else NTP)], out_dt) for _ in range(2)]
        tmps = [p.sb("tp", [128, 512], F32) for _ in range(2)]
        ch = [Chain(p, "ga"), Chain(p, "gb")]
        cx = p.sem("cx")
        XTv = fm(XT)
        Wv = [fm(w) for w in Ws]
        u = 0
        for pi in range(npass):
            ch[0].wait(nc.sync)
            ch[1].wait(nc.sync)
            cx.inc(nc.sync.dma_start(out=xs[:], in_=XTv[:, :, t0 + pi * NTP: t0 + (pi + 1) * NTP]), 16)
            if tokmajor:
                units = [(si, tb) for si in range(M // 512) for tb in range(NTP // 128)]
            else:
                units = [(mi, 0) for mi in range(M // 128)]
            prev_si = [None, None]
            for (a, b) in units:
                c = u % 2
                C = ch[c]
                b0 = 4 * c
                if tokmajor:
                    for w in range(nW):
                        C.dma(nc.gpsimd, ws[c][w][:], Wv[w][:, :, a * 512:(a + 1) * 512])
                    C.wait(nc.tensor)
                    nc.tensor.wait_ge(cx.h, cx.n)
                    for kc in range(Kc):
                        ins = nc.tensor.matmul(p.ps[:, b0, :], xs[:, kc, b * 128:(b + 1) * 128], ws[c][0][:, kc, :],
                                               start=(kc == 0), stop=(kc == Kc - 1))
                    C.s.inc(ins)
                    epi(C, [p.ps[:, b0, :]], outs[c][:], tmps[c], pi, a, b)
                    d = dst(pi, a, b)
                    C.dma(nc.sync, d, outs[c][:])
                else:
                    for w in range(nW):
                        C.dma(nc.gpsimd, ws[c][w][:], Wv[w][:, :, a * 128:(a + 1) * 128])
                    C.wait(nc.tensor)
                    nc.tensor.wait_ge(cx.h, cx.n)
                    for w in range(nW):
                        for j in range(nb):
                            for kc in range(Kc):
                                ins = nc.tensor.matmul(p.ps[:, b0 + w * nb + j, :], ws[c][w][:, kc, :],
                                                       xs[:, kc, j * 512:(j + 1) * 512],
                                                       start=(kc == 0), stop=(kc == Kc - 1))
                    C.s.inc(ins)
                    for j in range(nb):
                        epi(C, [p.ps[:, b0 + w * nb + j, :] for w in range(nW)], outs[c][:, j * 512:(j + 1) * 512],
                            tmps[c], pi, a, j)
                    if dst is not None:
                        C.dma(nc.sync, dst(a, pi), outs[c][:])
                u += 1
        for e in (nc.sync, nc.gpsimd, nc.tensor, nc.vector, nc.scalar):
            ch[0].wait(e)
            ch[1].wait(e)
        nc.all_engine_barrier()


def gemm2(p, XT, K, t0, ntok, Ws, M, body, dsts, NTP, out_dt=BF16, auxsrcs=None, aux_dts=(), pass_aux=None):
    nc = p.nc
    Kc = K // 128
    nW = len(Ws)
    NTP = min(NTP, ntok)
    nbs = min(2 // nW, NTP // 512)
    assert ntok % NTP == 0 and NTP % (512 * nbs) == 0
    npass = ntok // NTP
    nblk = NTP // 512
    nch = 4 if Kc <= 16 else 2
    nax = len(aux_dts)
    osz = 2 if out_dt == BF16 else 4
    asz = sum(2 if d == BF16 else 4 for d in aux_dts)
    est = lambda wb: Kc * NTP * 2 + nch * wb * nW * Kc * 256 + nch * NTP * (osz + asz) + nch * 2048 + (NTP * 4 if pass_aux else 0)
    wbuf = 2 if est(2) <= 148 * 1024 else 1
    nM = M // 128
    with nc.cleanup_on_exit():
        xs = p.sb("xs", [128, Kc, NTP], BF16)
        ws = [[[p.sb("ws", [128, Kc, 128], BF16) for _ in range(nW)] for _ in range(wbuf)] for _ in range(nch)]
        outs = [p.sb("ot", [128, NTP], out_dt) for _ in range(nch)]
        axs = [[p.sb("ax", [128, NTP], aux_dts[i]) for i in range(nax)] for _ in range(nch)]
        tmps = [p.sb("tp", [128, 512], F32) for _ in range(nch)]
        pax = p.sb("pax", [128, NTP], F32) if pass_aux is not None else None
        ch = [Chain(p, f"g{i}") for i in range(nch)]
        wl = [p.sem(f"wl{i}") for i in range(nch)]
        al = [p.sem(f"al{i}") for i in range(nch)]
        cx = p.sem("cx")
        XTv = fm(XT)
        Wv = [fm(w) for w in Ws]
        T, G = nc.tensor, nc.gpsimd
        groups = [(pi, list(range(m0, min(m0 + nch, nM)))) for pi in range(npass) for m0 in range(0, nM, nch)]
        mmdone = []

        def emit_wloads(gi):
            pi, grp = groups[gi]
            for idx, mi in enumerate(grp):
                if gi >= wbuf and idx < len(mmdone[gi - wbuf]):
                    G.wait_ge(ch[idx].s.h, mmdone[gi - wbuf][idx])
                for w in range(nW):
                    wl[idx].inc(G.dma_start(out=ws[idx][gi % wbuf][w][:], in_=Wv[w][:, :, mi * 128:(mi + 1) * 128]), 16)

        emit_wloads(0)
        last_pi = -1
        for gi, (pi, grp) in enumerate(groups):
            if pi != last_pi:
                for C in ch:
                    C.wait(nc.sync)
                cx.inc(nc.sync.dma_start(out=xs[:], in_=XTv[:, :, t0 + pi * NTP: t0 + (pi + 1) * NTP]), 16)
                if pass_aux is not None:
                    cx.inc(nc.sync.dma_start(out=pax[:], in_=pass_aux(pi, NTP)), 16)
                last_pi = pi
                newpass = True
            if nax:
                for idx, mi in enumerate(grp):
                    ch[idx].wait(nc.sync)
                    for i, s_ in enumerate(auxsrcs(mi, pi, NTP)):
                        al[idx].inc(nc.sync.dma_start(out=axs[idx][i][:], in_=s_), 16)
            wl_need = [wl[idx].n for idx in range(len(grp))]
            done = [0] * len(grp)
            for sbk in range(nblk // nbs):
                for idx, mi in enumerate(grp):
                    C = ch[idx]
                    b0 = 2 * idx
                    C.wait(T)
                    if sbk == 0:
                        T.wait_ge(wl[idx].h, wl_need[idx])
                        T.wait_ge(cx.h, cx.n)
                    for w in range(nW):
                        for jj in range(nbs):
                            blk = sbk * nbs + jj
                            for kc in range(Kc):
                                ins = T.matmul(p.ps[:, b0 + w * (2 // nW) + jj, :], ws[idx][gi % wbuf][w][:, kc, :],
                                               xs[:, kc, blk * 512:(blk + 1) * 512], start=(kc == 0), stop=(kc == Kc - 1))
                    done[idx] = C.s.inc(ins)
                    for jj in range(nbs):
                        blk = sbk * nbs + jj
                        sl = slice(blk * 512, (blk + 1) * 512)
                        if sbk == 0 and jj == 0:
                            for e_ in (nc.vector, nc.scalar):
                                if pass_aux is not None:
                                    e_.wait_ge(cx.h, cx.n)
                                if nax:
                                    e_.wait_ge(al[idx].h, al[idx].n)
                        body(C, [p.ps[:, b0 + w * (2 // nW) + jj, :] for w in range(nW)], outs[idx][:, sl],
                             [a_[:, sl] for a_ in axs[idx]], tmps[idx], mi, pi, blk, (pax[:, sl] if pax is not None else None))
            mmdone.append(done)
            if gi + 1 < len(groups):
                emit_wloads(gi + 1)
            for idx, mi in enumerate(grp):
                for d in dsts(mi, pi, NTP):
                    ch[idx].dma(nc.sync, d, outs[idx][:])
        for e in (nc.sync, nc.gpsimd, nc.tensor, nc.vector, nc.scalar):
            for C in ch:
                C.wait(e)
        nc.all_engine_barrier()


def gemm_tokmajor(p, XT, K, t0, ntok, Wm, M, OUT, NTP=2048):
    nc = p.nc
    Kc = K // 128
    NTP = min(NTP, ntok)
    npass = ntok // NTP
    nch = 4
    T, V = nc.tensor, nc.vector
    with nc.cleanup_on_exit():
        xs = p.sb("xs", [128, Kc, NTP], BF16)
        wsl = p.sb("wsl", [128, Kc, 512], BF16)
        outs = [p.sb("ot", [128, 512], BF16) for _ in range(nch)]
        ch = [Chain(p, f"t{i}") for i in range(nch)]
        cx = p.sem("cx")
        XTv, Wv = fm(XT), fm(Wm)
        u = 0
        for pi in range(npass):
            for C in ch:
                C.wait(nc.sync)
            cx.inc(nc.sync.dma_start(out=xs[:], in_=XTv[:, :, t0 + pi * NTP: t0 + (pi + 1) * NTP]), 16)
            for si in range(M // 512):
                for C in ch:
                    C.wait(nc.gpsimd)
                cx.inc(nc.gpsimd.dma_start(out=wsl[:], in_=Wv[:, :, si * 512:(si + 1) * 512]), 16)
                for tb in range(NTP // 128):
                    c = u % nch
                    C = ch[c]
                    C.wait(T)
                    T.wait_ge(cx.h, cx.n)
                    for kc in range(Kc):
                        ins = T.matmul(p.ps[:, 2 * c, :], xs[:, kc, tb * 128:(tb + 1) * 128], wsl[:, kc, :],
                                       start=(kc == 0), stop=(kc == Kc - 1))
                    C.s.inc(ins)
                    C.go(V, lambda: V.tensor_copy(out=outs[c][:], in_=p.ps[:, 2 * c, :]))
                    r0 = t0 + pi * NTP + tb * 128
                    C.dma(nc.sync, OUT[r0:r0 + 128, si * 512:(si + 1) * 512], outs[c][:])
                    u += 1
        for e in (nc.sync, nc.gpsimd, T, V):
            for C in ch:
                C.wait(e)
        nc.all_engine_barrier()


def layernorm(p, RT, g_col, b_col, t0, ntok, OUT32, OUTB):
    nc, cfg = p.nc, p.cfg
    Dc = cfg.D // 128
    NL = 256
    npass = ntok // NL
    NCH = 3
    with nc.cleanup_on_exit():
        ch = [Chain(p, f"l{i}") for i in range(NCH)]
        bufs = []
        for c in range(NCH):
            bufs.append(dict(r=p.sb("lnr", [128, Dc, NL], F32), sq=p.sb("lnsq", [128, Dc, NL], F32),
                             mean=p.sb("lnm", [128, NL], F32), rstd=p.sb("lnrs", [128, NL], F32),
                             ob=p.sb("lnob", [128, Dc, NL], BF16)))
        RTv, O32v, OBv = fm(RT), fm(OUT32), fm(OUTB)
        V, A, T = nc.vector, nc.scalar, nc.tensor
        for pi in range(npass):
            c = pi % NCH
            C, B = ch[c], bufs[c]
            sl = slice(t0 + pi * NL, t0 + (pi + 1) * NL)
            r, sq, mean, rstd, ob = B["r"], B["sq"], B["mean"], B["rstd"], B["ob"]
            C.dma(nc.sync, r[:], RTv[:, :, sl])
            C.go(A, lambda: A.activation(out=sq[:], in_=r[:], func=AF.Square))
            C.wait(T)
            for kc in range(Dc):
                T.matmul(p.ps[:, 2 * c, 0:NL], p.ones32[:], r[:, kc, :], start=(kc == 0), stop=(kc == Dc - 1))
            for kc in range(Dc):
                ins = T.matmul(p.ps[:, 2 * c + 1, 0:NL], p.ones32[:], sq[:, kc, :], start=(kc == 0), stop=(kc == Dc - 1))
            C.s.inc(ins)
            C.go(V, lambda: V.tensor_scalar(out=mean[:], in0=p.ps[:, 2 * c, 0:NL], scalar1=1.0 / cfg.D, scalar2=None, op0=ALU.mult))
            C.go(V, lambda: V.tensor_tensor(out=rstd[:], in0=mean[:], in1=mean[:], op=ALU.mult))
            C.go(V, lambda: V.scalar_tensor_tensor(out=rstd[:], in0=p.ps[:, 2 * c + 1, 0:NL], scalar=1.0 / cfg.D, in1=rstd[:],
                                                   op0=ALU.mult, op1=ALU.subtract))
            C.go(A, lambda: A.activation(out=rstd[:], in_=rstd[:], func=AF.Sqrt, bias=p.epsc[:, 0:1]))
            C.go(V, lambda: V.reciprocal(out=rstd[:], in_=rstd[:]))
            C.wait(V)
            for kc in range(Dc):
                ins = V.tensor_tensor(out=sq[:, kc, :], in0=r[:, kc, :], in1=mean[:], op=ALU.subtract)
            C.s.inc(ins)
            C.wait(V)
            for kc in range(Dc):
                ins = V.tensor_tensor(out=sq[:, kc, :], in0=sq[:, kc, :], in1=rstd[:], op=ALU.mult)
            C.s.inc(ins)
            C.wait(A)
            for kc in range(Dc):
                ins = A.activation(out=r[:, kc, :], in_=sq[:, kc, :], func=AF.Identity,
                                   scale=g_col[:, kc:kc + 1], bias=b_col[:, kc:kc + 1])
            C.s.inc(ins)
            C.dma(nc.sync, O32v[:, :, sl], r[:])
            v_id = C.s.n - 16
            V.wait_ge(C.s.h, v_id)
            C.s.inc(V.tensor_copy(out=ob[:], in_=r[:]))
            C.dma(nc.sync, OBv[:, :, sl], ob[:])
        for e in (nc.sync, nc.gpsimd, T, V, A):
            for C in ch:
                C.wait(e)
        nc.all_engine_barrier()


def pool_stage(p, UT32, t0, ntok, ZT):
    nc, cfg = p.nc, p.cfg
    npass = ntok // 512
    nch = cfg.PW // 128
    W = 528
    with nc.cleanup_on_exit():
        ch = [Chain(p, "pa"), Chain(p, "pb")]
        bufs = [dict(u=p.sb("pu", [128, W], F32), a=p.sb("pa", [128, W], F32), b=p.sb("pb", [128, W], F32),
                     val=p.sb("pv", [128, W], F32), ic=p.sb("pi", [128, 512], F32), z=p.sb("pz", [128, 512], BF16))
                for _ in range(2)]
        V = nc.vector
        Uv = fm(UT32)
        Zv = fm(ZT)
        un = 0
        for pi in range(npass):
            for ci in range(nch):
                g = ci // (cfg.PG // 128)
                w = 2 << g
                c = un % 2
                C, B = ch[c], bufs[c]
                u, sa, sbb, val, ic, z = B["u"], B["a"], B["b"], B["val"], B["ic"], B["z"]
                a0 = t0 + pi * 512
                if a0 == 0:
                    C.go(V, lambda: V.memset(u[:, 0:16], 0.0))
                    C.go(V, lambda: V.memset(val[:, 0:16], 0.0))
                    C.dma(nc.sync, u[:, 16:W], Uv[:, ci, 0:512])
                    C.dma(nc.sync, val[:, 16:W], bcast_row(p.tokvalid[0:1, 0:512], 512))
                else:
                    C.dma(nc.sync, u[:], Uv[:, ci, a0 - 16:a0 + 512])
                    C.dma(nc.sync, val[:], bcast_row(p.tokvalid[0:1, a0 - 16:a0 + 512], W))
                C.dma(nc.sync, ic[:], bcast_row(p.invcnt[g:g + 1, a0:a0 + 512], 512))
                C.go(V, lambda: V.tensor_tensor(out=u[:], in0=u[:], in1=val[:], op=ALU.mult))
                src = u
                lo = 0
                step = 1
                dsts = [sa, sbb]
                k = 0
                while step < w:
                    d = dsts[k % 2]
                    C.go(V, lambda: V.tensor_tensor(out=d[:, lo + step:W], in0=src[:, lo + step:W],
                                                    in1=src[:, lo:W - step], op=ALU.add))
                    src = d
                    lo += step
                    step *= 2
                    k += 1
                d = dsts[k % 2]
                C.go(V, lambda: V.tensor_tensor(out=d[:, 16:W], in0=src[:, 16:W], in1=ic[:], op=ALU.mult))
                C.go(V, lambda: V.tensor_tensor(out=z[:], in0=d[:, 16:W], in1=u[:, 16:W], op=ALU.subtract))
                C.dma(nc.sync, Zv[:, ci, a0:a0 + 512], z[:])
                un += 1
        for e in (nc.sync, V):
            ch[0].wait(e)
            ch[1].wait(e)
        nc.all_engine_barrier()


def attention(p, l, QT, KT, V_tm, t0, ntok, AOT, lam_col, sub_col):
    nc, cfg = p.nc, p.cfg
    P = cfg.P
    nkb = P // 128
    T, V, A = nc.tensor, nc.vector, nc.scalar
    with nc.cleanup_on_exit():
        kt = p.sb("kt", [128, P], BF16)
        vt = p.sb("vt", [128, nkb, 128], BF16)
        qt = p.sb("qt", [128, ntok], BF16)
        btp = p.sb("btp", [128, 1024], BF16)
        pt = [p.sb("pt", [128, 2, 512], BF16) for _ in range(2)]
        rz = [p.sb("rz", [128, 512], F32) for _ in range(2)]
        o = p.sb("o", [128, 512], F32)
        o1 = p.sb("o1", [128, 512], F32)
        sq = p.sb("osq", [128, 512], F32)
        ao = p.sb("ao", [128, 512], BF16)
        ch = [Chain(p, f"c{i}") for i in range(2)]
        E = Chain(p, "ep")
        hx = p.sem("hx")
        for h in range(cfg.H):
            for C in ch + [E]:
                C.wait(nc.sync)
            hx.inc(nc.sync.dma_start(out=kt[:], in_=KT[h * 128:(h + 1) * 128, :]), 16)
            hx.inc(nc.sync.dma_start(out=vt[:], in_=V_tm[:, h * 128:(h + 1) * 128].rearrange("(kb k) e -> k kb e", k=128)), 16)
            hx.inc(nc.sync.dma_start(out=qt[:], in_=QT[h * 128:(h + 1) * 128, t0:t0 + ntok]), 16)
            hx.inc(nc.sync.dma_start(out=btp[:], in_=p.BTP[h]), 16)
            for qi in range(ntok // 512):
                gq = (t0 // 512) + qi
                nk = 4 * gq + 4
                def emit_scores(kb):
                    jr = kb - 4 * gq
                    near = jr >= -1
                    x = kb % 2
                    C = ch[x]
                    C.wait(T)
                    T.wait_ge(hx.h, hx.n)
                    if kb == 0:
                        E.wait(T)
                    for c in range(2):
                        ins = T.matmul(p.ps[:, 2 * x + c, :], kt[64 * c:64 * c + 64, kb * 128:(kb + 1) * 128],
                                       qt[64 * c:64 * c + 64, qi * 512:(qi + 1) * 512], start=True, stop=not near)
                        if near:
                            off = 384 - 128 * jr
                            ins = T.matmul(p.ps[:, 2 * x + c, :], p.Jm[:], btp[:, off:off + 512], start=False, stop=True)
                    C.s.inc(ins)

                emit_scores(0)
                for kb in range(nk):
                    jr = kb - 4 * gq
                    near = jr >= -1
                    x = kb % 2
                    C = ch[x]
                    if kb + 1 < nk:
                        emit_scores(kb + 1)
                    bias = p.kvb[:, kb:kb + 1] if near else p.farb[:, h * nkb + kb:h * nkb + kb + 1]
                    C.go(A, lambda: A.activation(out=pt[x][:], in_=p.ps[:, 2 * x:2 * x + 2, :], func=AF.Exp, scale=0.125, bias=bias))
                    C.wait(T)
                    if kb == 0:
                        E.wait(T)
                    for c in range(2):
                        T.matmul(p.ps[:, 4 + c, :], vt[:, kb, :], pt[x][:, c, :], start=(kb == 0), stop=(kb == nk - 1))
                        ins = T.matmul(p.ps[:, 6 + c, :], p.onesb[:], pt[x][:, c, :], start=(kb == 0), stop=(kb == nk - 1))
                    C.s.inc(ins)
                for C in ch:
                    C.wait(V)
                for c in range(2):
                    E.go(V, lambda: V.tensor_scalar(out=rz[c][:], in0=p.ps[:, 6 + c, :], scalar1=1e-30, scalar2=None, op0=ALU.add))
                    E.go(V, lambda: V.reciprocal(out=rz[c][:], in_=rz[c][:]))
                E.go(V, lambda: V.tensor_tensor(out=o[:], in0=p.ps[:, 4, :], in1=rz[0][:], op=ALU.mult))
                E.go(V, lambda: V.tensor_tensor(out=o1[:], in0=p.ps[:, 5, :], in1=rz[1][:], op=ALU.mult))
                E.go(V, lambda: V.scalar_tensor_tensor(out=o[:], in0=o1[:], scalar=lam_col[:, 0:1], in1=o[:], op0=ALU.mult, op1=ALU.add))
                E.go(V, lambda: V.tensor_tensor(out=sq[:], in0=o[:], in1=o[:], op=ALU.mult))
                E.go(T, lambda: T.matmul(p.ps[:, 0, :], p.ones32[:], sq[:], start=True, stop=True))
                E.go(V, lambda: V.tensor_scalar(out=sq[:], in0=p.ps[:, 0, :], scalar1=1.0 / 128, scalar2=None, op0=ALU.mult))
                E.go(A, lambda: A.activation(out=sq[:], in_=sq[:], func=AF.Sqrt, bias=p.epsc[:, 0:1]))
                E.go(V, lambda: V.reciprocal(out=sq[:], in_=sq[:]))
                E.go(V, lambda: V.tensor_tensor(out=o[:], in0=o[:], in1=sq[:], op=ALU.mult))
                E.go(V, lambda: V.tensor_scalar(out=ao[:], in0=o[:], scalar1=sub_col[:, 0:1], scalar2=None, op0=ALU.mult))
                E.dma(nc.sync, AOT[h * 128:(h + 1) * 128, t0 + qi * 512:t0 + (qi + 1) * 512], ao[:])
        for e in (nc.sync, T, V, A):
            for C in ch + [E]:
                C.wait(e)
        nc.all_engine_barrier()


def setup_consts(p, l_count):
    nc, cfg = p.nc, p.cfg
    P, H = cfg.P, cfg.H
    nkb = P // 128
    Dc = cfg.D // 128
    V, A, T = nc.vector, nc.scalar, nc.tensor
    C = Chain(p, "su")
    p.ones32 = p.sb("ones32", [128, 128], F32)
    p.onesb = p.sb("onesb", [128, 128], BF16)
    p.Jm = p.sb("Jm", [128, 128], BF16)
    p.kvb = p.sb("kvb", [128, nkb], F32)
    p.farb = p.sb("farb", [128, H * nkb], F32)
    p.lncols = {}
    p.epsc = p.sb("epsc", [128, 1], F32)
    C.go(V, lambda: V.memset(p.epsc[:], 1e-5))
    C.go(V, lambda: V.memset(p.ones32[:], 1.0))
    C.go(V, lambda: V.memset(p.onesb[:], 1.0))
    C.dma(nc.gpsimd, p.Jm[:], p.J_in[:, :])
    C.dma(nc.sync, p.kvb[:], bcast_row(p.kvalid[0:1, :], nkb))
    for l in range(l_count):
        for nm, src in (("g1", p.w["ln1_g"]), ("b1", p.w["ln1_b"]), ("g2", p.w["ln2_g"]), ("b2", p.w["ln2_b"])):
            t = p.sb(f"{nm}{l}", [128, Dc], F32)
            C.dma(nc.sync, t[:], src[l].rearrange("(c q) -> q c", q=128), allow_slow_non_contiguous=True)
            p.lncols[(nm, l)] = t
        t = p.sb(f"psc{l}", [128, cfg.PW // 128], F32)
        C.dma(nc.sync, t[:], p.w["pool_scale"][l].rearrange("(c q) -> q c", q=128), allow_slow_non_contiguous=True)
        p.lncols[("psc", l)] = t
        li = 0.8 - 0.6 * math.exp(-0.3 * l)
        t = p.sb(f"sub{l}", [128, 1], F32)
        C.dma(nc.sync, t[:], p.w["subln_w"][l].rearrange("(q o) -> q o", o=1), allow_slow_non_contiguous=True)
        C.go(V, lambda: V.tensor_scalar(out=t[:], in0=t[:], scalar1=1.0 - li, scalar2=None, op0=ALU.mult))
        p.lncols[("sub", l)] = t
        lv = p.sb(f"lv{l}", [128, 4, 64], F32)
        C.dma(nc.sync, lv[:], bass.AP(p.w["lambdas"][l].tensor, p.w["lambdas"][l].offset, [[0, 128], [64, 4], [1, 64]]))
        pr = p.sb(f"lpr{l}", [128, 2, 64], F32)
        C.go(V, lambda: V.tensor_tensor(out=pr[:, 0, :], in0=lv[:, 0, :], in1=lv[:, 1, :], op=ALU.mult))
        C.go(V, lambda: V.tensor_tensor(out=pr[:, 1, :], in0=lv[:, 2, :], in1=lv[:, 3, :], op=ALU.mult))
        sm = p.sb(f"lsm{l}", [128, 2], F32)
        C.go(V, lambda: V.tensor_reduce(out=sm[:], in_=pr[:], axis=AX.X, op=ALU.add))
        C.go(A, lambda: A.activation(out=sm[:], in_=sm[:], func=AF.Exp))
        lam = p.sb(f"lam{l}", [128, 1], F32)
        C.go(V, lambda: V.tensor_tensor(out=lam[:], in0=sm[:, 1:2], in1=sm[:, 0:1], op=ALU.subtract))
        C.go(V, lambda: V.tensor_scalar(out=lam[:], in0=lam[:], scalar1=-li, scalar2=None, op0=ALU.add))
        p.lncols[("nlam", l)] = lam
    rb31 = p.sb("rb31", [128, H], F32)
    C.dma(nc.sync, rb31[:], bcast_row(p.w["rel_bias"][31:32, :], H))
    for h in range(H):
        C.go(V, lambda: V.tensor_scalar(out=p.farb[:, h * nkb:(h + 1) * nkb], in0=p.kvb[:], scalar1=rb31[:, h:h + 1],
                                        scalar2=None, op0=ALU.add))
    rba = p.sb("rba", [33, H], F32)
    oh = p.sb("oh", [33, cfg.GW], F32)
    gsb = p.sb("gsb", [H, cfg.GW], BF16)
    C.go(V, lambda: V.memset(rba[:], 1.0))
    C.dma(nc.sync, rba[0:32, :], p.w["rel_bias"][:, :])
    C.dma(nc.sync, oh[:], p.OH_in[:, :])
    for j in range(0, cfg.GW, 384):
        C.go(T, lambda: T.matmul(p.ps[0:H, 0, 0:384], rba[:], oh[:, j:j + 384], start=True, stop=True))
        C.go(V, lambda: V.tensor_scalar(out=gsb[:, j:j + 384], in0=p.ps[0:H, 0, 0:384], scalar1=8.0, scalar2=None, op0=ALU.mult))
    C.dma(nc.sync, p.Gd[:, :], gsb[:])
    C.wait(nc.gpsimd)
    C.wait(nc.scalar)
    bts = p.sem("bts")
    engs = [nc.sync, nc.gpsimd, nc.scalar]
    i = 0
    for h in range(H):
        for k in range(128):
            e = engs[i % 3]
            bts.inc(e.dma_start(out=p.BTP[h, k:k + 1, :], in_=p.Gd[h:h + 1, k:k + 1024]), 16)
            i += 1
    for e in (nc.sync, nc.gpsimd, T, V, A):
        C.wait(e)
        e.wait_ge(bts.h, bts.n)
    nc.all_engine_barrier()


def router_stage(p, l, H32, t0, ntok, GT):
    nc, cfg = p.nc, p.cfg
    Dc = cfg.D // 128
    NE = cfg.NE
    V, A, T = nc.vector, nc.scalar, nc.tensor
    with nc.cleanup_on_exit():
        C = Chain(p, "rt")
        rw = p.sb("rw", [128, Dc, NE], F32)
        hs = p.sb("hs", [128, Dc, 512], F32)
        lg = p.sb("lg", [128, NE], F32)
        l2 = p.sb("l2", [128, NE], F32)
        e1 = p.sb("e1", [128, NE], F32)
        e2 = p.sb("e2", [128, NE], F32)
        m1 = p.sb("m1", [128, 1], F32)
        m2 = p.sb("m2", [128, 1], F32)
        g1 = p.sb("g1", [128, 1], F32)
        g2 = p.sb("g2", [128, 1], F32)
        C.dma(nc.sync, rw[:], p.w["router_w"][l // 2].rearrange("(c q) e -> q c e", q=128))
        Hv = fm(H32)
        for pi in range(ntok // 512):
            C.dma(nc.sync, hs[:], Hv[:, :, t0 + pi * 512:t0 + (pi + 1) * 512])
            for tb in range(4):
                C.wait(T)
                for kc in range(Dc):
                    ins = T.matmul(p.ps[:, 0, 0:NE], hs[:, kc, tb * 128:(tb + 1) * 128], rw[:, kc, :],
                                   start=(kc == 0), stop=(kc == Dc - 1))
                C.s.inc(ins)
                C.go(V, lambda: V.tensor_copy(out=lg[:], in_=p.ps[:, 0, 0:NE]))
                C.go(V, lambda: V.tensor_reduce(out=m1[:], in_=lg[:], axis=AX.X, op=ALU.max))
                C.go(V, lambda: V.tensor_scalar(out=e1[:], in0=lg[:], scalar1=m1[:, 0:1], scalar2=None, op0=ALU.is_equal))
                C.go(V, lambda: V.scalar_tensor_tensor(out=l2[:], in0=e1[:], scalar=-1e30, in1=lg[:], op0=ALU.mult, op1=ALU.add))
                C.go(V, lambda: V.tensor_reduce(out=m2[:], in_=l2[:], axis=AX.X, op=ALU.max))
                C.go(V, lambda: V.tensor_scalar(out=e2[:], in0=l2[:], scalar1=m2[:, 0:1], scalar2=None, op0=ALU.is_equal))
                C.go(V, lambda: V.tensor_tensor(out=g2[:], in0=m1[:], in1=m2[:], op=ALU.subtract))
                C.go(A, lambda: A.activation(out=g1[:], in_=g2[:], func=AF.Sigmoid))
                C.go(V, lambda: V.tensor_scalar(out=g2[:], in0=g1[:], scalar1=-1.0, scalar2=1.0, op0=ALU.mult, op1=ALU.add))
                C.go(V, lambda: V.tensor_scalar(out=e1[:], in0=e1[:], scalar1=g1[:, 0:1], scalar2=None, op0=ALU.mult))
                C.go(V, lambda: V.scalar_tensor_tensor(out=e1[:], in0=e2[:], scalar=g2[:, 0:1], in1=e1[:], op0=ALU.mult, op1=ALU.add))
                a0 = pi * 512 + tb * 128
                C.dma(nc.sync, GT[:, a0:a0 + 128].rearrange("e t -> t e"), e1[:], allow_slow_non_contiguous=True)
        for e in (nc.sync, T, V, A):
            C.wait(e)
        nc.all_engine_barrier()


def build(cfg):
    p = Prog(cfg)
    nc = p.nc
    D, P, OWN, H, AW, PW = cfg.D, cfg.P, cfg.OWN, cfg.H, cfg.AW, cfg.PW
    L = cfg.DEPTH
    NDn, NMo = (L + 1) // 2, L // 2
    w = {}
    w["xT"] = p.din("xT", [D, P])
    w["w_in"] = p.din("w_in", [L, D, cfg.INW])
    w["lambdas"] = p.din("lambdas", [L, 4, 64])
    w["subln_w"] = p.din("subln_w", [L, 128])
    w["pool_w"] = p.din("pool_w", [L, 4, cfg.PG, cfg.PG])
    w["pool_scale"] = p.din("pool_scale", [L, PW])
    w["w_branch_attn"] = p.din("w_branch_attn", [L, AW, D])
    w["w_branch_pool"] = p.din("w_branch_pool", [L, PW, D])
    w["w_out"] = p.din("w_out", [L, D, D])
    w["rel_bias"] = p.din("rel_bias", [32, H])
    for nm in ("ln1_g", "ln1_b", "ln2_g", "ln2_b"):
        w[nm] = p.din(nm, [L, D])
    w["dense_w_gate"] = p.din("dense_w_gate", [NDn, D, cfg.DFF])
    w["dense_w_up"] = p.din("dense_w_up", [NDn, D, cfg.DFF])
    w["dense_w_down"] = p.din("dense_w_down", [NDn, cfg.DFF, D])
    w["router_w"] = p.din("router_w", [NMo, D, cfg.NE])
    w["moe_w_gate"] = p.din("moe_w_gate", [NMo, cfg.NE, D, cfg.DFE])
    w["moe_w_up"] = p.din("moe_w_up", [NMo, cfg.NE, D, cfg.DFE])
    w["moe_w_down"] = p.din("moe_w_down", [NMo, cfg.NE, cfg.DFE, D])
    p.w = w
    p.kvalid = p.din("kvalid", [1, P // 128])
    p.tokvalid = p.din("tokvalid", [1, P])
    p.invcnt = p.din("invcnt", [4, P])
    p.OH_in = p.din("OH", [33, cfg.GW])
    p.J_in = p.din("Jm", [128, 128])
    yT = nc.dram_tensor("yT", [D, OWN], F32, kind="ExternalOutput").ap()
    p.Gd = p.dram("Gd", [H, cfg.GW], BF16)
    p.BTP = p.dram("BTP", [H, 128, 1024], BF16)
    XB = p.dram("XB", [D, P], BF16)
    QT = p.dram("QT", [AW, P], BF16)
    KT = p.dram("KT", [AW, P], BF16)
    Vt = p.dram("Vt", [P, AW], BF16)
    UT = p.dram("UT", [PW, P], F32)
    SGA = p.dram("SGA", [D, P], BF16)
    SGP = p.dram("SGP", [D, P], BF16)
    AOT = p.dram("AOT", [AW, P], BF16)
    ZT = p.dram("ZT", [PW, P], BF16)
    POT = p.dram("POT", [PW, P], BF16)
    MT = p.dram("MT", [D, P], BF16)
    RT = p.dram("RT", [D, P], F32)
    H32 = p.dram("H32", [D, P], F32)
    HB = p.dram("HB", [D, P], BF16)
    X32 = p.dram("X32", [D, P], F32)
    HE = p.dram("HE", [max(cfg.DFF, cfg.DFE), P], BF16)
    GT = p.dram("GT", [cfg.NE, OWN], F32)
    FA = p.dram("FA", [D, OWN], F32)
    p.ps = nc.alloc_psum_tensor("ps", [128, 8, 512], F32)
    V, A, T = nc.vector, nc.scalar, nc.tensor

    setup_consts(p, L)

    with nc.cleanup_on_exit():
        C = Chain(p, "xc")
        step = min(D, max(128, (1 << 22) // (P * 4) // 128 * 128))
        for r0 in range(0, D, step):
            C.dma(nc.gpsimd, XB[r0:r0 + step, :], w["xT"][r0:r0 + step, :])
        C.wait(nc.gpsimd)
        nc.all_engine_barrier()

    Xin32 = w["xT"]
    for l in range(L):
        t0 = 0 if l == 0 else P - OWN
        ntok = P - t0
        wi = w["w_in"][l]
        o_q, o_k, o_v, o_u, o_ga, o_gp = 0, AW, 2 * AW, 3 * AW, 3 * AW + PW, 3 * AW + PW + D
        NTq = min(2048, ntok)

        def ecopy(C, ps, out, aux, tmp, *a):
            C.go(V, lambda: V.tensor_copy(out=out, in_=ps[0]))

        def esig(C, ps, out, aux, tmp, *a):
            C.go(A, lambda: A.activation(out=out, in_=ps[0], func=AF.Sigmoid))

        def dcols(DST, toff):
            return lambda mi, pi, n: [DST[mi * 128:(mi + 1) * 128, toff + pi * n:toff + (pi + 1) * n]]

        gemm2(p, XB, D, t0, ntok, [wi[:, o_q:o_q + AW]], AW, ecopy, dcols(QT, t0), 2048)
        gemm2(p, XB, D, 0, P, [wi[:, o_k:o_k + AW]], AW, ecopy, dcols(KT, 0), 2048)
        gemm_tokmajor(p, XB, D, 0, P, wi[:, o_v:o_v + AW], AW, Vt)
        tu = max(0, t0 - 512)
        gemm2(p, XB, D, tu, P - tu, [wi[:, o_u:o_u + PW]], PW, ecopy, dcols(UT, tu), (2048 if (P - tu) % 2048 == 0 else 512), out_dt=F32)
        gemm2(p, XB, D, t0, ntok, [wi[:, o_ga:o_ga + D]], D, esig, dcols(SGA, t0), 2048)
        gemm2(p, XB, D, t0, ntok, [wi[:, o_gp:o_gp + D]], D, esig, dcols(SGP, t0), 2048)
        attention(p, l, QT, KT, Vt, t0, ntok, AOT, p.lncols[("nlam", l)], p.lncols[("sub", l)])
        pool_stage(p, UT, t0, ntok, ZT)
        psc = p.lncols[("psc", l)]
        for g in range(4):
            PG = cfg.PG

            def epool(C, ps, out, aux, tmp, mi, pi, blk, pax, g=g):
                col = g * (PG // 128) + mi
                C.go(V, lambda: V.tensor_scalar(out=out, in0=ps[0], scalar1=psc[:, col:col + 1], scalar2=None, op0=ALU.mult))
            gemm2(p, ZT[g * PG:(g + 1) * PG, :], PG, t0, ntok, [w["pool_w"][l, g]], PG, epool,
                  (lambda mi, pi, n, g=g: [POT[g * PG + mi * 128:g * PG + (mi + 1) * 128, t0 + pi * n:t0 + (pi + 1) * n]]), 2048)
        merge_gemm(p, AOT, AW, t0, ntok, w["w_branch_attn"][l], SGA, None, MT)
        merge_gemm(p, POT, PW, t0, ntok, w["w_branch_pool"][l], SGP, MT, MT)
        resid_gemm(p, MT, D, t0, ntok, [w["w_out"][l]], Xin32, RT, cfg.ALPHA, 0)
        layernorm(p, RT, p.lncols[("g1", l)], p.lncols[("b1", l)], t0, ntok, H32, HB)
        if l % 2 == 0:
            DFF = cfg.DFF

            def eswi(C, ps, out, aux, tmp, *a):
                C.go(A, lambda: A.activation(out=tmp[:], in_=ps[0], func=AF.Silu))
                C.go(V, lambda: V.tensor_tensor(out=out, in0=tmp[:], in1=ps[1], op=ALU.mult))
            gemm2(p, HB, D, t0, ntok, [w["dense_w_gate"][l // 2], w["dense_w_up"][l // 2]], DFF, eswi, dcols(HE, t0), 2048)
            resid_gemm(p, HE[0:DFF, :], DFF, t0, ntok, [w["dense_w_down"][l // 2]], H32, RT, cfg.ALPHA, 0, NTP=1024)
        else:
            router_stage(p, l, H32, t0, ntok, GT)
            for e in range(cfg.NE):
                moe_up_gemm(p, HB, t0, ntok, w["moe_w_gate"][l // 2, e], w["moe_w_up"][l // 2, e], GT, e, HE)
                resid_gemm(p, HE[0:cfg.DFE, :], cfg.DFE, t0, ntok, [w["moe_w_down"][l // 2, e]],
                           H32 if e == 0 else None, FA, cfg.ALPHA, t0, acc_prev=(e > 0), final_dst=(RT if e == cfg.NE - 1 else None), NTP=1024)
        if l == L - 1:
            layernorm(p, RT, p.lncols[("g2", l)], p.lncols[("b2", l)], t0, ntok, X32, XB)
            with nc.cleanup_on_exit():
                C = Chain(p, "oc")
                for r0 in range(0, D, 128):
                    C.dma(nc.sync, yT[r0:r0 + 128, :], X32[r0:r0 + 128, P - OWN:P])
                C.wait(nc.sync)
                nc.all_engine_barrier()
        else:
            layernorm(p, RT, p.lncols[("g2", l)], p.lncols[("b2", l)], t0, ntok, X32, XB)
            Xin32 = X32
    return p


def merge_gemm(p, XT, K, t0, ntok, Wm, SG, PREV, OUT):
    V = p.nc.vector
    D = p.cfg.D

    def srcs(mi, pi, n):
        s = [SG[mi * 128:(mi + 1) * 128, t0 + pi * n:t0 + (pi + 1) * n]]
        if PREV is not None:
            s.append(PREV[mi * 128:(mi + 1) * 128, t0 + pi * n:t0 + (pi + 1) * n])
        return s

    def body(C, ps, out, ax, tmp, mi, pi, blk, pax):
        if PREV is None:
            C.go(V, lambda: V.tensor_tensor(out=out, in0=ps[0], in1=ax[0], op=ALU.mult))
        else:
            C.go(V, lambda: V.tensor_tensor(out=tmp[:], in0=ps[0], in1=ax[0], op=ALU.mult))
            C.go(V, lambda: V.tensor_tensor(out=out, in0=tmp[:], in1=ax[1], op=ALU.add))
    gemm2(p, XT, K, t0, ntok, [Wm], D, body,
          lambda mi, pi, n: [OUT[mi * 128:(mi + 1) * 128, t0 + pi * n:t0 + (pi + 1) * n]], 2048, out_dt=BF16,
          auxsrcs=srcs, aux_dts=([BF16] if PREV is None else [BF16, BF16]))


def resid_gemm(p, XT, K, t0, ntok, Ws, RES32, OUT, alpha, o0, acc_prev=False, final_dst=None, NTP=None):
    V = p.nc.vector
    D = p.cfg.D
    if NTP is None:
        NTP = 1024 if K // 128 <= 16 else 512

    def srcs(mi, pi, n):
        s = []
        if RES32 is not None:
            s.append(RES32[mi * 128:(mi + 1) * 128, t0 + pi * n:t0 + (pi + 1) * n])
        if acc_prev:
            s.append(OUT[mi * 128:(mi + 1) * 128, t0 - o0 + pi * n:t0 - o0 + (pi + 1) * n])
        return s

    def body(C, ps, out, ax, tmp, mi, pi, blk, pax):
        i = 0
        cur = ps[0]
        if RES32 is not None:
            C.go(V, lambda: V.scalar_tensor_tensor(out=out, in0=ax[0], scalar=alpha, in1=cur, op0=ALU.mult, op1=ALU.add))
            cur = out
            i = 1
        if acc_prev:
            cc = cur
            C.go(V, lambda: V.tensor_tensor(out=out, in0=cc, in1=ax[i], op=ALU.add))
            cur = out
        if cur is ps[0]:
            C.go(V, lambda: V.tensor_copy(out=out, in_=ps[0]))

    def dst(mi, pi, n):
        d = [OUT[mi * 128:(mi + 1) * 128, t0 - o0 + pi * n:t0 - o0 + (pi + 1) * n]]
        if final_dst is not None:
            d.append(final_dst[mi * 128:(mi + 1) * 128, t0 + pi * n:t0 + (pi + 1) * n])
        return d
    nax = (RES32 is not None) + (1 if acc_prev else 0)
    gemm2(p, XT, K, t0, ntok, [Ws[0]], D, body, dst, NTP, out_dt=F32, auxsrcs=srcs, aux_dts=[F32] * nax)


def moe_up_gemm(p, HB, t0, ntok, Wg, Wu, GT, e, HE):
    nc, cfg = p.nc, p.cfg
    V, A = nc.vector, nc.scalar

    def body(C, ps, out, ax, tmp, mi, pi, blk, pax):
        C.go(A, lambda: A.activation(out=tmp[:], in_=ps[0], func=AF.Silu))
        C.go(V, lambda: V.tensor_tensor(out=tmp[:], in0=tmp[:], in1=ps[1], op=ALU.mult))
        C.go(V, lambda: V.tensor_tensor(out=out, in0=tmp[:], in1=pax, op=ALU.mult))
    gemm2(p, HB, cfg.D, t0, ntok, [Wg, Wu], cfg.DFE, body,
          lambda mi, pi, n: [HE[mi * 128:(mi + 1) * 128, t0 + pi * n:t0 + (pi + 1) * n]], 2048, out_dt=BF16,
          pass_aux=lambda pi, n: bcast_row(GT[e:e + 1, pi * n:(pi + 1) * n], n))


def t5_bucket_np(n):
    n = np.maximum(n, 0)
    large = 16 + (np.log(np.maximum(n, 1).astype(np.float32) / 16) / math.log(128 / 16) * 16).astype(np.int32)
    large = np.minimum(large, 31)
    return np.where(n < 16, n, large)


def host_consts(cfg, r):
    P, OWN = cfg.P, cfg.OWN
    nreal = OWN * (r + 1)
    pad = P - nreal
    kvalid = np.zeros((1, P // 128), np.float32)
    kvalid[0, :pad // 128] = NEG
    tokvalid = np.zeros((1, P), np.float32)
    tokvalid[0, pad:] = 1.0
    invcnt = np.zeros((4, P), np.float32)
    t = np.arange(nreal)
    for g, wdw in enumerate((2, 4, 8, 16)):
        invcnt[g, pad:] = 1.0 / np.minimum(t + 1, wdw).astype(np.float32)
    n = np.arange(cfg.GW) - 511
    OH = np.zeros((33, cfg.GW), np.float32)
    b = t5_bucket_np(n)
    ok = n >= 0
    OH[b[ok], np.nonzero(ok)[0]] = 1.0
    OH[32, ~ok] = NEG
    J = np.zeros((128, 128), np.float32)
    J[np.arange(128), 127 - np.arange(128)] = 1.0
    return dict(kvalid=kvalid, tokvalid=tokvalid, invcnt=invcnt, OH=OH, Jm=J)


_CACHE = {}


def run(cfg, inputs):
    key = id(cfg)
    if key not in _CACHE:
        _CACHE[key] = build(cfg)
    p = _CACHE[key]
    x = np.asarray(inputs["x"], np.float32)
    ncore = 4 * cfg.BATCH
    in_maps = []
    for c in range(ncore):
        b, r = divmod(c, 4)
        nreal = cfg.OWN * (r + 1)
        xp = np.zeros((cfg.P, cfg.D), np.float32)
        xp[cfg.P - nreal:] = x[b, :nreal]
        m = {"xT": np.ascontiguousarray(xp.T)}
        for k, v in inputs.items():
            if k != "x":
                m[k] = np.asarray(v, np.float32)
        m.update(host_consts(cfg, r))
        in_maps.append(m)
    res = run_bass_kernel_spmd(p.nc, in_maps, core_ids=list(range(ncore)))
    out = np.zeros((cfg.BATCH, 4 * cfg.OWN, cfg.D), np.float32)
    for c in range(ncore):
        b, r = divmod(c, 4)
        out[b, r * cfg.OWN:(r + 1) * cfg.OWN] = res.results[c]["yT"].T
    return out


FULL = Cfg()


def kernel(**inputs):
    return run(FULL, inputs)
```
